# Optimizing a Trainium2 kernel written in Bass

```python
import jax, jax.numpy as jnp
from jax import lax
import numpy as np

D_MODEL = 1024
BATCH = 1
SEQ = 16384
DEPTH = 4

HEAD_DIM = 64
A_HEADS = 8
B_HEADS = 4
C_HEADS = 4
A_WIDTH = A_HEADS * HEAD_DIM
B_WIDTH = B_HEADS * HEAD_DIM
C_WIDTH = C_HEADS * HEAD_DIM
MIX_WIDTH = A_WIDTH + B_WIDTH + C_WIDTH
MOBA_BLOCK = 256
MOBA_TOPK = 3
MOBA_QCHUNK = 128
RET_CHUNK = 256
MLSTM_CHUNK = 128
PAD_MULT = 256
CONV_K = 4
RMS_EPS = 1e-6
HEAD_NORM_EPS = 1e-5
ROPE_BASE = 10000.0
SPLIT_SIZES = [A_WIDTH] * 4 + [B_WIDTH] * 4 + [C_WIDTH] * 5 + [C_HEADS, C_HEADS]
N_IN = sum(SPLIT_SIZES)

kernel_name = "hymba_style_moba_retention_mlstm_trunk"


def rmsnorm(x, w):
    x32 = x.astype(jnp.float32)
    y = x32 * lax.rsqrt(jnp.mean(x32 * x32, axis=-1, keepdims=True) + RMS_EPS)
    return (y * w.astype(jnp.float32)).astype(x.dtype)


def split_heads(t, n_heads):
    b, s, _ = t.shape
    return t.reshape(b, s, n_heads, HEAD_DIM).transpose(0, 2, 1, 3)


def merge_heads(t):
    b, h, s, d = t.shape
    return t.transpose(0, 2, 1, 3).reshape(b, s, h * d)


def head_norm(y, w):
    mu = jnp.mean(y, axis=-1, keepdims=True)
    var = jnp.mean(jnp.square(y - mu), axis=-1, keepdims=True)
    y = (y - mu) * lax.rsqrt(var + HEAD_NORM_EPS)
    return merge_heads(y) * w.astype(jnp.float32)


def alibi_slopes(n_heads):
    return jnp.exp2(-8.0 * jnp.arange(1, n_heads + 1, dtype=jnp.float32) / n_heads)


def rotate(t, pos):
    half = t.shape[-1] // 2
    theta = 1.0 / (ROPE_BASE ** jnp.linspace(0.0, 1.0, half, dtype=jnp.float32))
    ang = pos[:, None] * theta[None, :]
    cos, sin = jnp.cos(ang), jnp.sin(ang)
    t1, t2 = t[..., :half], t[..., half:]
    return jnp.concatenate([t1 * cos - t2 * sin, t1 * sin + t2 * cos], axis=-1)


def moba_attention(q, k, v):
    b, h, s, dh = q.shape
    nb = s // MOBA_BLOCK
    n_sel = min(MOBA_TOPK, nb - 1)
    scale = dh ** -0.5
    slopes = alibi_slopes(h)[None, :, None, None]
    kb = k.reshape(b, h, nb, MOBA_BLOCK, dh)
    vb = v.reshape(b, h, nb, MOBA_BLOCK, dh)
    kmean = jnp.mean(kb, axis=3)
    bi = jnp.arange(b)[:, None, None, None]
    hi = jnp.arange(h)[None, :, None, None]
    offs = jnp.arange(MOBA_BLOCK)
    qloc = jnp.arange(MOBA_QCHUNK)

    def one_chunk(c):
        t0 = c * MOBA_QCHUNK
        qc = lax.dynamic_slice_in_dim(q, t0, MOBA_QCHUNK, axis=2)
        tpos = t0 + qloc
        j = t0 // MOBA_BLOCK
        k_own = lax.dynamic_slice_in_dim(k, j * MOBA_BLOCK, MOBA_BLOCK, axis=2)
        v_own = lax.dynamic_slice_in_dim(v, j * MOBA_BLOCK, MOBA_BLOCK, axis=2)
        dist_own = tpos[:, None] - (j * MOBA_BLOCK + offs)[None, :]
        s_own = jnp.einsum('bhqd,bhkd->bhqk', qc, k_own) * scale - slopes * dist_own
        s_own = jnp.where(dist_own >= 0, s_own, -jnp.inf)
        if n_sel == 0:
            p = jax.nn.softmax(s_own, axis=-1)
            return jnp.einsum('bhqk,bhkd->bhqd', p, v_own)
        gate = jnp.einsum('bhqd,bhnd->bhqn', qc, kmean)
        gate = jnp.where(jnp.arange(nb) < j, gate, -jnp.inf)
        _, idx = lax.top_k(gate, n_sel)
        valid = jnp.arange(n_sel) < j
        k_sel = kb[bi, hi, idx]
        v_sel = vb[bi, hi, idx]
        dist = tpos[:, None, None] - (idx[..., None] * MOBA_BLOCK + offs)
        s_sel = jnp.einsum('bhqd,bhqnkd->bhqnk', qc, k_sel) * scale - slopes[..., None] * dist
        s_sel = jnp.where(valid[:, None], s_sel, -jnp.inf)
        n_k = n_sel * MOBA_BLOCK
        s_all = jnp.concatenate([s_sel.reshape(b, h, MOBA_QCHUNK, n_k), s_own], axis=-1)
        p = jax.nn.softmax(s_all, axis=-1)
        p_sel = p[..., :n_k].reshape(b, h, MOBA_QCHUNK, n_sel, MOBA_BLOCK)
        p_own = p[..., n_k:]
        return (jnp.einsum('bhqnk,bhqnkd->bhqd', p_sel, v_sel)
                + jnp.einsum('bhqk,bhkd->bhqd', p_own, v_own))

    outs = lax.map(one_chunk, jnp.arange(s // MOBA_QCHUNK))
    return outs.transpose(1, 2, 0, 3, 4).reshape(b, h, s, dh)


def retention(q, k, v):
    b, h, s, dh = q.shape
    L = RET_CHUNK
    nc = s // L
    log_gamma = jnp.log(1.0 - jnp.exp2(-5.0 - jnp.arange(h, dtype=jnp.float32)))
    k = k * (dh ** -0.5)
    qc = q.reshape(b, h, nc, L, dh)
    kc = k.reshape(b, h, nc, L, dh)
    vc = v.reshape(b, h, nc, L, dh)
    pos = jnp.arange(L, dtype=jnp.float32)
    diff = pos[:, None] - pos[None, :]
    decay = jnp.where(diff >= 0, jnp.exp(log_gamma[:, None, None] * jnp.maximum(diff, 0.0)), 0.0)
    scores = jnp.einsum('bhcnd,bhcmd->bhcnm', qc, kc) * decay[None, :, None]
    o_intra = jnp.einsum('bhcnm,bhcme->bhcne', scores, vc)
    zeta = jnp.exp(log_gamma[:, None] * (L - 1.0 - pos))
    kv = jnp.einsum('bhcmd,bhcme->bhcde', kc * zeta[None, :, None, :, None], vc)
    chunk_decay = jnp.exp(log_gamma * L)[None, :, None, None]

    def step(state, kv_c):
        return chunk_decay * state + kv_c, state

    r0 = jnp.zeros((b, h, dh, dh), jnp.float32)
    _, r_prev = lax.scan(step, r0, kv.transpose(2, 0, 1, 3, 4))
    r_prev = r_prev.transpose(1, 2, 0, 3, 4)
    xi = jnp.exp(log_gamma[:, None] * (pos + 1.0))
    o_cross = jnp.einsum('bhcnd,bhcde->bhcne', qc, r_prev) * xi[None, :, None, :, None]
    return (o_intra + o_cross).reshape(b, h, s, dh)


def mlstm(q, k, v, i_pre, f_pre):
    b, h, s, dh = q.shape
    L = MLSTM_CHUNK
    nc = s // L
    k = k * (dh ** -0.5)
    to_chunks = lambda t: t.reshape(b, h, nc, L, dh).transpose(2, 0, 1, 3, 4)
    qc, kc, vc = to_chunks(q), to_chunks(k), to_chunks(v)
    ic = i_pre.reshape(b, h, nc, L).transpose(2, 0, 1, 3)
    bc = jnp.cumsum(jax.nn.log_sigmoid(f_pre).reshape(b, h, nc, L), axis=-1).transpose(2, 0, 1, 3)
    pos = jnp.arange(L)
    causal = pos[:, None] >= pos[None, :]

    def step(carry, inp):
        c_st, n_st, m_st = carry
        qt, kt, vt, it, bt = inp
        d_log = jnp.where(causal, bt[..., :, None] - bt[..., None, :] + it[..., None, :], -jnp.inf)
        inter_log = bt + m_st[..., None]
        m_t = jnp.maximum(inter_log, jnp.max(d_log, axis=-1))
        w_intra = jnp.exp(d_log - m_t[..., None])
        w_inter = jnp.exp(inter_log - m_t)
        sc = jnp.einsum('bhtd,bhsd->bhts', qt, kt) * w_intra
        num = (jnp.einsum('bhts,bhse->bhte', sc, vt)
               + w_inter[..., None] * jnp.einsum('bhtd,bhde->bhte', qt, c_st))
        den = jnp.sum(sc, axis=-1) + w_inter * jnp.einsum('bhtd,bhd->bht', qt, n_st)
        h_t = num / jnp.maximum(jnp.abs(den), jnp.exp(-m_t))[..., None]
        b_last = bt[..., -1]
        g = b_last[..., None] - bt + it
        m_new = jnp.maximum(b_last + m_st, jnp.max(g, axis=-1))
        a = jnp.exp(b_last + m_st - m_new)
        w = jnp.exp(g - m_new[..., None])
        c_new = a[..., None, None] * c_st + jnp.einsum('bhs,bhsd,bhse->bhde', w, kt, vt)
        n_new = a[..., None] * n_st + jnp.einsum('bhs,bhsd->bhd', w, kt)
        return (c_new, n_new, m_new), h_t

    init = (jnp.zeros((b, h, dh, dh), jnp.float32), jnp.zeros((b, h, dh), jnp.float32),
            jnp.zeros((b, h), jnp.float32))
    _, hs = lax.scan(step, init, (qc, kc, vc, ic, bc))
    return hs.transpose(1, 2, 0, 3, 4).reshape(b, h, s, dh)


def causal_dwconv(x, w, bias):
    c = x.shape[-1]
    y = lax.conv_general_dilated(x, w.astype(x.dtype)[:, None, :], window_strides=(1,),
                                 padding=[(CONV_K - 1, 0)], dimension_numbers=('NWC', 'WIO', 'NWC'),
                                 feature_group_count=c)
    return y + bias.astype(x.dtype)


def hybrid_layer(x, norm_w, w_in, conv_w, conv_b, gate_bias, ret_norm_w, mlstm_norm_w, w_out):
    b, s, _ = x.shape
    s_pad = -(-s // PAD_MULT) * PAD_MULT
    h = rmsnorm(x, norm_w)
    proj = jnp.matmul(h, w_in).astype(jnp.float32)
    proj = jnp.pad(proj, ((0, 0), (0, s_pad - s), (0, 0)))
    cuts = np.cumsum(SPLIT_SIZES)[:-1].tolist()
    (aq, ak, av, ag, bq, bk, bv, bg, cq, ck, cv, co, cg, ci, cf) = jnp.split(proj, cuts, axis=-1)

    ya = moba_attention(split_heads(aq, A_HEADS), split_heads(ak, A_HEADS), split_heads(av, A_HEADS))
    ya = merge_heads(ya) * jax.nn.silu(ag)

    pos = jnp.arange(s_pad, dtype=jnp.float32)
    yb = retention(rotate(split_heads(bq, B_HEADS), pos), rotate(split_heads(bk, B_HEADS), pos),
                   split_heads(bv, B_HEADS))
    yb = head_norm(yb, ret_norm_w) * jax.nn.silu(bg)

    qk = jax.nn.silu(causal_dwconv(jnp.concatenate([cq, ck], axis=-1), conv_w, conv_b))
    cq_c, ck_c = qk[..., :C_WIDTH], qk[..., C_WIDTH:]
    gb = gate_bias.astype(jnp.float32)
    i_pre = ci.transpose(0, 2, 1) + gb[:C_HEADS][None, :, None]
    f_pre = cf.transpose(0, 2, 1) + gb[C_HEADS:][None, :, None]
    yc = mlstm(split_heads(cq_c, C_HEADS), split_heads(ck_c, C_HEADS), split_heads(cv, C_HEADS),
               i_pre, f_pre)
    yc = yc * split_heads(jax.nn.sigmoid(co), C_HEADS)
    yc = head_norm(yc, mlstm_norm_w) * jax.nn.silu(cg)

    y = jnp.concatenate([ya, yb, yc], axis=-1)[:, :s].astype(x.dtype)
    return x + jnp.matmul(y, w_out)


def setup_inputs(seed: int = 0) -> dict:
    key = jax.random.key(seed)
    ks = jax.random.split(key, 12)
    f32 = jnp.float32
    x = jax.random.normal(ks[0], (BATCH, SEQ, D_MODEL), f32)
    norm_w = 1.0 + 0.02 * jax.random.normal(ks[1], (DEPTH, D_MODEL), f32)
    w_in = jax.random.normal(ks[2], (DEPTH, D_MODEL, N_IN), f32) * D_MODEL ** -0.5
    conv_w = jax.random.normal(ks[3], (DEPTH, CONV_K, 2 * C_WIDTH), f32) * CONV_K ** -0.5
    conv_b = 0.02 * jax.random.normal(ks[4], (DEPTH, 2 * C_WIDTH), f32)
    i_bias = 0.1 * jax.random.normal(ks[5], (DEPTH, C_HEADS), f32)
    f_bias = jnp.linspace(3.0, 6.0, C_HEADS, dtype=f32)[None, :] + 0.1 * jax.random.normal(ks[6], (DEPTH, C_HEADS), f32)
    gate_bias = jnp.concatenate([i_bias, f_bias], axis=-1)
    ret_norm_w = 1.0 + 0.02 * jax.random.normal(ks[7], (DEPTH, B_WIDTH), f32)
    mlstm_norm_w = 1.0 + 0.02 * jax.random.normal(ks[8], (DEPTH, C_WIDTH), f32)
    w_out = jax.random.normal(ks[9], (DEPTH, MIX_WIDTH, D_MODEL), f32) * MIX_WIDTH ** -0.5
    final_norm_w = 1.0 + 0.02 * jax.random.normal(ks[10], (D_MODEL,), f32)
    return {"x": x, "norm_w": norm_w, "w_in": w_in, "conv_w": conv_w, "conv_b": conv_b,
            "gate_bias": gate_bias, "ret_norm_w": ret_norm_w, "mlstm_norm_w": mlstm_norm_w,
            "w_out": w_out, "final_norm_w": final_norm_w}


def reference(x, norm_w, w_in, conv_w, conv_b, gate_bias, ret_norm_w, mlstm_norm_w, w_out, final_norm_w):
    for layer in range(DEPTH):
        x = hybrid_layer(x, norm_w[layer], w_in[layer], conv_w[layer], conv_b[layer], gate_bias[layer],
                         ret_norm_w[layer], mlstm_norm_w[layer], w_out[layer])
    return rmsnorm(x, final_norm_w)
```

```python
import contextlib
import numpy as np
import ml_dtypes
import concourse.bass as bass
import concourse.mybir as mybir
from concourse.bass_utils import run_bass_kernel_spmd

F32 = mybir.dt.float32
BF16 = mybir.dt.bfloat16
ALU = mybir.AluOpType
AF = mybir.ActivationFunctionType
AX = mybir.AxisListType
NPBF = ml_dtypes.bfloat16

NCORES = 8
S = 16384
D = 1024
DEPTH = 4
TOK = S // NCORES
RMS_EPS = 1e-6


class Buf:
    __slots__ = ("w", "r", "name", "psum")

    def __init__(self, name="", psum=False):
        self.w = None
        self.r = []
        self.name = name
        self.psum = psum


class Prog:
    CENG = ("pe", "act", "dve", "pool")
    QENG = ("sp",)
    NDMA = 24

    def __init__(self, nc, stack):
        self.nc = nc
        self.stack = stack
        self.sem = {e: stack.enter_context(nc.semaphore("s_" + e)) for e in self.CENG}
        self.cnt = {e: 0 for e in self.CENG}
        self.q = {e: [] for e in self.CENG + self.QENG}
        self.seen = {e: {} for e in self.CENG + self.QENG}
        self.needed = {e: set() for e in self.CENG}
        self.dsem = [stack.enter_context(nc.semaphore("s_dma%d" % i)) for i in range(self.NDMA)]
        self.dval = [0] * self.NDMA
        self.drr = 0
        self.nops = 0

    def sb(self, name, shape, dt):
        return self.stack.enter_context(self.nc.sbuf_tensor(name, shape, dt))

    def ps(self, name, shape, dt):
        return self.stack.enter_context(self.nc.psum_tensor(name, shape, dt))

    def _deps(self, reads, writes):
        ev = []
        for b in reads:
            if b.w is not None:
                ev.append(b.w)
        for b in writes:
            if b.w is not None:
                ev.append(b.w)
            ev.extend(b.r)
        return ev

    def _waits(self, eng, evs):
        need = {}
        for (key, val) in evs:
            if key == eng and eng == "pe":
                continue
            if need.get(key, 0) < val:
                need[key] = val
        for key, val in need.items():
            if self.seen[eng].get(key, 0) >= val:
                continue
            self.seen[eng][key] = val
            if key in self.needed:
                self.needed[key].add(val)
            self.q[eng].append(("wait", key, val))

    def _mark(self, ev, reads, writes):
        for b in reads:
            b.r.append(ev)
        for b in writes:
            b.w = ev
            b.r = []
        self.nops += 1

    def op(self, eng, fn, reads=(), writes=()):
        reads = [b for b in reads if b is not None]
        writes = [b for b in writes if b is not None]
        if eng != "pe":
            writes = writes + [b for b in reads if b.psum and b not in writes]
        self._waits(eng, self._deps(reads, writes))
        self.cnt[eng] += 1
        idx = self.cnt[eng]
        self.q[eng].append(("op", fn, idx))
        self._mark((eng, idx), reads, writes)

    def dma(self, out, in_, reads=(), writes=(), q="sp", **kw):
        reads = [b for b in reads if b is not None]
        writes = [b for b in writes if b is not None]
        i = self.drr
        self.drr = (i + 1) % self.NDMA
        evs = self._deps(reads, writes)
        key = "dma%d" % i
        if self.dval[i] > 0:
            evs.append((key, self.dval[i]))
        self._waits(q, evs)
        self.dval[i] += 16
        self.q[q].append(("dma", out, in_, i, kw))
        self._mark((key, self.dval[i]), reads, writes)

    def finish(self, out_bufs):
        evs = [b.w for b in out_bufs if b.w is not None]
        self._waits("sp", evs)
        rank = {}
        for e in self.CENG:
            rank[e] = {idx: k + 1 for k, idx in enumerate(sorted(self.needed[e]))}

        def emit(engname, e):
            for item in self.q[engname]:
                if item[0] == "wait":
                    _, key, val = item
                    if key in rank:
                        e.wait_ge(self.sem[key], rank[key][val])
                    else:
                        e.wait_ge(self.dsem[int(key[3:])], val)
                elif item[0] == "op":
                    _, fn, idx = item
                    ins = fn(e)
                    if idx in rank[engname]:
                        ins.then_inc(self.sem[engname], 1)
                else:
                    _, out, in_, i, kw = item
                    e.dma_start(out=out, in_=in_, **kw).then_inc(self.dsem[i], 16)

        with self.nc.Block() as block:
            @block.sync
            def _(e):
                emit("sp", e)

            @block.tensor
            def _(e):
                emit("pe", e)

            @block.scalar
            def _(e):
                emit("act", e)

            @block.vector
            def _(e):
                emit("dve", e)

            @block.gpsimd
            def _(e):
                emit("pool", e)


def bcast_rows(ap_1xn, nparts):
    t = ap_1xn.tensor
    n = ap_1xn.shape[-1]
    return bass.AP(t, ap_1xn.offset, [[0, nparts], [1, n]])


def build_pa(first, final):
    nc = bass.Bass("TRN2", target_bir_lowering=False)
    NT = TOK // 128
    x_d = nc.dram_tensor("x", [TOK, D], F32, kind="ExternalInput").ap()
    nw_d = nc.dram_tensor("nw", [1, D], F32, kind="ExternalInput").ap()
    id_d = nc.dram_tensor("ident", [128, 128], BF16, kind="ExternalInput").ap()
    if not first:
        yT_d = nc.dram_tensor("yT", [D, TOK], BF16, kind="ExternalInput").ap()
        wo_d = nc.dram_tensor("wo", [D, D], F32, kind="ExternalInput").ap()
        if not final:
            xo_d = nc.dram_tensor("xo", [TOK, D], F32, kind="ExternalOutput").ap()
    if final:
        out_d = nc.dram_tensor("out", [TOK, D], F32, kind="ExternalOutput").ap()
    else:
        hT_d = nc.dram_tensor("hT", [D, TOK], BF16, kind="ExternalOutput").ap()

    with contextlib.ExitStack() as st:
        P = Prog(nc, st)
        nwb = P.sb("nwb", [128, D], F32)
        ident = P.sb("ident_sb", [128, 128], BF16)
        b_nwb, b_id = Buf(), Buf()
        P.dma(nwb[:], bcast_rows(nw_d, 128), writes=[b_nwb])
        P.dma(ident[:], id_d, writes=[b_id])
        NB = 2
        xt = [P.sb("xt%d" % i, [128, D], F32) for i in range(NB)]
        b_xt = [Buf() for _ in range(NB)]
        junk = P.sb("junk", [128, D], F32)
        b_junk = Buf()
        ss = [P.sb("ss%d" % i, [128, 4], F32) for i in range(NB)]
        b_ss = [Buf() for _ in range(NB)]
        if not final:
            hb = [P.sb("hb%d" % i, [128, D], BF16) for i in range(NB)]
            b_hb = [Buf() for _ in range(NB)]
            hTs = P.sb("hTs", [128, 8, TOK], BF16)
            b_hTs = Buf()
            pt = [P.ps("pt%d" % i, [128, 1024], BF16) for i in range(2)]
            b_pt = [Buf(psum=True) for _ in range(2)]
        else:
            ho = [P.sb("ho%d" % i, [128, D], F32) for i in range(NB)]
            b_ho = [Buf() for _ in range(NB)]
        b_out = Buf()
        if not first:
            yTs = P.sb("yTs", [128, 8, TOK], BF16)
            b_yTs = Buf()
            P.dma(yTs[:], yT_d.rearrange("(mc p) t -> p mc t", p=128), writes=[b_yTs])
            wob = P.sb("wob", [128, 8, D], BF16)
            b_wob = Buf()
            wst = [P.sb("wst%d" % i, [128, D], F32) for i in range(2)]
            b_wst = [Buf() for _ in range(2)]
            for mc in range(8):
                k = mc % 2
                P.dma(wst[k][:], wo_d[mc * 128:(mc + 1) * 128, :], writes=[b_wst[k]])
                P.op("pool", lambda e, mc=mc, k=k: e.tensor_copy(out=wob[:, mc, :], in_=wst[k][:]),
                     reads=[b_wst[k]], writes=[b_wob])
            pacc = [P.ps("pacc%d" % i, [128, 512], F32) for i in range(4)]
            b_pacc = [Buf(psum=True) for _ in range(4)]
            b_xo = Buf()

        for tt in range(NT):
            k = tt % NB
            tsl = slice(tt * 128, (tt + 1) * 128)
            P.dma(xt[k][:], x_d[tsl, :], writes=[b_xt[k]])
            if not first:
                for half in range(2):
                    pa_i = (tt * 2 + half) % 4
                    for mc in range(8):
                        P.op("pe", lambda e, mc=mc, half=half, pa_i=pa_i, tsl=tsl: e.matmul(
                            pacc[pa_i][:], lhsT=yTs[:, mc, tsl], rhs=wob[:, mc, half * 512:(half + 1) * 512],
                            start=(mc == 0), stop=(mc == 7)),
                            reads=[b_yTs, b_wob], writes=[b_pacc[pa_i]])
                    P.op("dve", lambda e, k=k, half=half, pa_i=pa_i: e.tensor_tensor(
                        out=xt[k][:, half * 512:(half + 1) * 512], in0=xt[k][:, half * 512:(half + 1) * 512],
                        in1=pacc[pa_i][:], op=ALU.add),
                        reads=[b_pacc[pa_i], b_xt[k]], writes=[b_xt[k]])
                if not final:
                    P.dma(xo_d[tsl, :], xt[k][:], reads=[b_xt[k]], writes=[b_xo])
            P.op("act", lambda e, k=k: e.activation(out=junk[:], in_=xt[k][:], func=AF.Square,
                                                     accum_out=ss[k][:, 0:1]),
                 reads=[b_xt[k]], writes=[b_junk, b_ss[k]])
            P.op("dve", lambda e, k=k: e.tensor_scalar(out=ss[k][:, 1:2], in0=ss[k][:, 0:1], scalar1=1.0 / D,
                                                       scalar2=RMS_EPS, op0=ALU.mult, op1=ALU.add),
                 reads=[b_ss[k]], writes=[b_ss[k]])
            P.op("act", lambda e, k=k: e.activation(out=ss[k][:, 2:3], in_=ss[k][:, 1:2], func=AF.Sqrt),
                 reads=[b_ss[k]], writes=[b_ss[k]])
            P.op("dve", lambda e, k=k: e.reciprocal(out=ss[k][:, 3:4], in_=ss[k][:, 2:3]),
                 reads=[b_ss[k]], writes=[b_ss[k]])
            if final:
                P.op("dve", lambda e, k=k: e.scalar_tensor_tensor(
                    out=ho[k][:], in0=xt[k][:], scalar=ss[k][:, 3:4], in1=nwb[:], op0=ALU.mult, op1=ALU.mult),
                    reads=[b_xt[k], b_ss[k], b_nwb], writes=[b_ho[k]])
                P.dma(out_d[tsl, :], ho[k][:], reads=[b_ho[k]], writes=[b_out])
            else:
                P.op("dve", lambda e, k=k: e.scalar_tensor_tensor(
                    out=hb[k][:], in0=xt[k][:], scalar=ss[k][:, 3:4], in1=nwb[:], op0=ALU.mult, op1=ALU.mult),
                    reads=[b_xt[k], b_ss[k], b_nwb], writes=[b_hb[k]])
                pk = tt % 2
                for mc in range(8):
                    P.op("pe", lambda e, k=k, mc=mc, pk=pk: e.transpose(
                        out=pt[pk][:, mc * 128:(mc + 1) * 128], in_=hb[k][:, mc * 128:(mc + 1) * 128],
                        identity=ident[:]),
                        reads=[b_hb[k], b_id], writes=[b_pt[pk]])
                P.op("act", lambda e, pk=pk, tsl=tsl: e.copy(
                    out=hTs[:, :, tsl], in_=pt[pk][:].rearrange("p (m t) -> p m t", m=8)),
                    reads=[b_pt[pk]], writes=[b_hTs])
        outs = []
        if not final:
            P.dma(hT_d.rearrange("(mc p) t -> p mc t", p=128), hTs[:], reads=[b_hTs], writes=[b_out])
            outs.append(b_out)
            if not first:
                outs.append(b_xo)
        else:
            outs.append(b_out)
        P.finish(outs)
    return nc


def _ident():
    return np.eye(128, dtype=np.float32).astype(NPBF)


BIG = 30000.0
NBLK = S // 256
NCH = S // 128
NG = S // 512


def build_pb(do_a=True, do_bc=True, S=S, stopA=0, flags=()):
    NCH = S // 128
    NG = S // 512
    nc = bass.Bass("TRN2", target_bir_lowering=False)
    hT_d = nc.dram_tensor("hT", [D, S], BF16, kind="ExternalInput").ap()
    wA_d = nc.dram_tensor("wA", [D, 384], F32, kind="ExternalInput").ap()
    id_d = nc.dram_tensor("ident", [128, 128], BF16, kind="ExternalInput").ap()
    E_d = nc.dram_tensor("Emat", [64, S], BF16, kind="ExternalInput").ap()
    tri_d = nc.dram_tensor("tri", [128, 128], BF16, kind="ExternalInput").ap()
    caus_d = nc.dram_tensor("caus", [128, 128], F32, kind="ExternalInput").ap()
    btab_d = nc.dram_tensor("btab", [128, 128], F32, kind="ExternalInput").ap()
    negb_d = nc.dram_tensor("negb", [128, 4], F32, kind="ExternalInput").ap()
    yT_d = nc.dram_tensor("yT", [192, S], BF16, kind="ExternalOutput").ap()
    wT_d = nc.dram_tensor("wT", [D, 512], F32, kind="ExternalInput").ap()
    wR_d = nc.dram_tensor("wR", [D, 322], F32, kind="ExternalInput").ap()
    tabs_d = nc.dram_tensor("tabs", [4, 64, S], F32, kind="ExternalInput").ap()
    cw_d = nc.dram_tensor("cw", [64, 10], F32, kind="ExternalInput").ap()
    misc_d = nc.dram_tensor("misc", [128, 8], F32, kind="ExternalInput").ap()
    wcol_d = nc.dram_tensor("wcol", [128, 1], F32, kind="ExternalInput").ap()
    trif_d = nc.dram_tensor("trif", [128, 128], F32, kind="ExternalInput").ap()
    idf_d = nc.dram_tensor("identf", [128, 128], F32, kind="ExternalInput").ap()
    hT_v = hT_d.rearrange("(mc p) t -> p mc t", p=128)
    gC_d = nc.dram_tensor("gC_scratch", [S // 128, 128, 128], BF16, kind="Internal").ap()
    lo = slice(0, 64)
    hi = slice(64, 128)

    with contextlib.ExitStack() as st:
        P = Prog(nc, st)
        b_y = Buf()
        ident = P.sb("ident_sb", [128, 128], BF16); b_id = Buf()
        P.dma(ident[:], id_d, writes=[b_id])
        tri = P.sb("tri_sb", [128, 128], BF16); b_tri = Buf()
        P.dma(tri[:], tri_d, writes=[b_tri])
        caus = P.sb("caus_sb", [128, 128], F32); b_caus = Buf()
        P.dma(caus[:], caus_d, writes=[b_caus])
        btab = P.sb("btab_sb", [128, 128], F32); b_btab = Buf()
        P.dma(btab[:], btab_d, writes=[b_btab])
        negb = P.sb("negb_sb", [128, 4], F32); b_negb = Buf()
        P.dma(negb[:], negb_d, writes=[b_negb])
        zeros = P.sb("zeros_sb", [128, 260], BF16); b_zero = Buf()
        P.op("dve", lambda e: e.memset(zeros[:], 0.0), writes=[b_zero])
        bank = [P.ps("bank%d" % i, [128, 512], F32) for i in range(6)]
        b_bank = [Buf(psum=True) for _ in range(6)]
        bankT = P.ps("bankT", [128, 1024], BF16); b_bankT = Buf(psum=True)
        hTt = [P.sb("hTt%d" % i, [128, 8, 512], BF16) for i in range(2)]
        b_hTt = [Buf() for _ in range(2)]
        wst = P.sb("wst", [128, 512], F32); b_wst = Buf()
        Wb = P.sb("Wb", [128, 8, 512], BF16); b_Wb = Buf()

        def load_w(dst, b_dst, src_d, ncols):
            for mc in range(8):
                P.dma(wst[:, 0:ncols], src_d[mc * 128:(mc + 1) * 128, :], writes=[b_wst])
                P.op("pool", lambda e, mc=mc: e.tensor_copy(out=dst[:, mc, 0:ncols], in_=wst[:, 0:ncols]),
                     reads=[b_wst], writes=[b_dst])

        QM = P.sb("QM", [128, S], BF16); b_QM = Buf()
        KE = P.sb("KE", [128, S], BF16); b_KE = Buf()
        VA = P.sb("VA", [128, NCH, 65], BF16); b_VA = Buf()
        P.op("dve", lambda e: e.memset(VA[:, :, 64:65], 1.0), writes=[b_VA])
        sgA = P.sb("sgA", [128, NCH, 64], BF16); b_sgA = Buf()

        if do_a:
            wA = Wb
            load_w(wA, b_Wb, wA_d, 384)
            if "noE" not in flags:
                P.dma(KE[hi, :], E_d, writes=[b_KE])
            kmean = P.sb("kmean", [128, 64], F32); b_km = Buf()
            P.op("dve", lambda e: e.memset(kmean[:], 0.0), writes=[b_km])
            q32 = [P.sb("q32_%d" % i, [128, 512], F32) for i in range(2)]
            b_q32 = [Buf() for _ in range(2)]
            gm = [P.sb("gm%d" % i, [128, 64], F32) for i in range(2)]
            sel = [P.sb("sel%d" % i, [128, 64], F32) for i in range(2)]
            top8 = [P.sb("top8_%d" % i, [128, 8], F32) for i in range(2)]
            thr = [P.sb("thr%d" % i, [128, 1], F32) for i in range(2)]
            negm = [P.sb("negm%d" % i, [128, 128], BF16) for i in range(2)]
            b_gm = [Buf() for _ in range(2)]
            b_sel = [Buf() for _ in range(2)]
            b_top8 = [Buf() for _ in range(2)]
            b_thr = [Buf() for _ in range(2)]
            b_negm = [Buf() for _ in range(2)]
            for i in range(2):
                P.op("dve", lambda e, i=i: e.memset(negm[i][:], 0.0), writes=[b_negm[i]])

            for i in range(NG):
                hb = i % 2
                tsl = slice(i * 512, (i + 1) * 512)
                P.dma(hTt[hb][:], hT_v[:, :, tsl], writes=[b_hTt[hb]])
                for mc in range(8 if "nok" not in flags else 0):
                    P.op("pe", lambda e, mc=mc, hb=hb: e.matmul(bank[1][:, :], lhsT=wA[:, mc, 128:256],
                                                               rhs=hTt[hb][:, mc, :], start=(mc == 0), stop=(mc == 7)),
                         reads=[b_Wb, b_hTt[hb]], writes=[b_bank[1]])
                if "nok" not in flags and "nokc" not in flags:
                  P.op("act", lambda e, tsl=tsl: e.copy(out=KE[lo, tsl], in_=bank[1][lo, :]),
                     reads=[b_bank[1]], writes=[b_KE])
                if "nok" not in flags and "nored" not in flags:
                  P.op("dve", lambda e, i=i: e.tensor_reduce(
                    out=kmean[lo, 2 * i:2 * i + 2], in_=bank[1][lo, :].rearrange("p (b t) -> p b t", b=2),
                    axis=AX.X, op=ALU.add),
                    reads=[b_bank[1]], writes=[b_km])
                for mc in range(8 if "noq" not in flags else 0):
                    P.op("pe", lambda e, mc=mc, hb=hb: e.matmul(bank[0][:, :], lhsT=wA[:, mc, 0:128],
                                                               rhs=hTt[hb][:, mc, :], start=(mc == 0), stop=(mc == 7)),
                         reads=[b_Wb, b_hTt[hb]], writes=[b_bank[0]])
                if "noq" not in flags:
                  P.op("act", lambda e, tsl=tsl: e.mul(out=QM[lo, tsl], in_=bank[0][lo, :], mul=0.125),
                     reads=[b_bank[0]], writes=[b_QM])
                qb = i % 2
                if "noq" not in flags:
                  P.op("dve", lambda e, qb=qb: e.tensor_copy(out=q32[qb][:], in_=bank[0][:, :]),
                     reads=[b_bank[0]], writes=[b_q32[qb]])
                for sub in range(4 if "norow" not in flags else 0):
                    ch = 4 * i + sub
                    rb = 2 + sub % 2
                    for mc in range(8):
                        P.op("pe", lambda e, mc=mc, hb=hb, sub=sub, rb=rb: e.matmul(
                            bank[rb][:, 0:128], lhsT=hTt[hb][:, mc, sub * 128:(sub + 1) * 128],
                            rhs=wA[:, mc, 256:384], start=(mc == 0), stop=(mc == 7)),
                            reads=[b_Wb, b_hTt[hb]], writes=[b_bank[rb]])
                    P.op("dve", lambda e, ch=ch, rb=rb: e.tensor_copy(out=VA[:, ch, 0:64], in_=bank[rb][:, 0:64]),
                         reads=[b_bank[rb]], writes=[b_VA])
                    P.op("act", lambda e, ch=ch, rb=rb: e.activation(out=sgA[:, ch, :], in_=bank[rb][:, 64:128],
                                                                       func=AF.Silu),
                         reads=[b_bank[rb]], writes=[b_sgA])
                for sub in range(4 if stopA != 1 else 0):
                    ch = 4 * i + sub
                    j = ch // 2
                    sb_ = ch % 2
                    P.op("pe", lambda e, qb=qb, sub=sub: e.matmul(
                        bank[4][:, 0:64], lhsT=q32[qb][:, sub * 128:(sub + 1) * 128], rhs=kmean[:, :],
                        start=True, stop=True),
                        reads=[b_q32[qb], b_km], writes=[b_bank[4]])
                    P.op("dve", lambda e, sb_=sb_, j=j: e.tensor_tensor(
                        out=gm[sb_][:], in0=bank[4][:, 0:64], in1=caus[:, 64 - j:128 - j], op=ALU.add),
                        reads=[b_bank[4], b_caus], writes=[b_gm[sb_]])
                    P.op("dve", lambda e, sb_=sb_: e.max(out=top8[sb_][:], in_=gm[sb_][:]),
                         reads=[b_gm[sb_]], writes=[b_top8[sb_]])
                    P.op("dve", lambda e, sb_=sb_: e.tensor_scalar(
                        out=thr[sb_][:], in0=top8[sb_][:, 2:3], scalar1=-1e29, scalar2=None, op0=ALU.max),
                        reads=[b_top8[sb_]], writes=[b_thr[sb_]])
                    P.op("dve", lambda e, sb_=sb_: e.tensor_scalar(
                        out=sel[sb_][:], in0=gm[sb_][:], scalar1=thr[sb_][:, 0:1], scalar2=None, op0=ALU.is_ge),
                        reads=[b_gm[sb_], b_thr[sb_]], writes=[b_sel[sb_]])
                    P.op("dve", lambda e, sb_=sb_, j=j: e.memset(sel[sb_][:, j:j + 1], 1.0),
                         reads=[b_sel[sb_]], writes=[b_sel[sb_]])
                    P.op("dve", lambda e, sb_=sb_, sub=sub: e.tensor_scalar(
                        out=negm[sb_][:, 64:128], in0=sel[sb_][:], scalar1=BIG, scalar2=negb[:, sub:sub + 1],
                        op0=ALU.mult, op1=ALU.add),
                        reads=[b_sel[sb_], b_negb], writes=[b_negm[sb_]])
                    tcol = (ch % 8) * 128
                    P.op("pe", lambda e, sb_=sb_, tcol=tcol: e.transpose(
                        out=bankT[:, tcol:tcol + 128], in_=negm[sb_][:], identity=ident[:]),
                        reads=[b_negm[sb_], b_id], writes=[b_bankT])
                    P.op("act", lambda e, ch=ch, tcol=tcol: e.copy(
                        out=QM[hi, ch * 128:(ch + 1) * 128], in_=bankT[hi, tcol:tcol + 128]),
                        reads=[b_bankT], writes=[b_QM])

            NPT = 3
            PT = [P.sb("PT%d" % i, [128, 512], BF16) for i in range(NPT)]
            b_PT = [Buf() for _ in range(NPT)]
            ya = [P.sb("ya%d" % i, [128, 4, 64], BF16) for i in range(2)]
            b_ya = [Buf() for _ in range(2)]
            rec = [P.sb("rec%d" % i, [128, 4], F32) for i in range(2)]
            b_rec = [Buf() for _ in range(2)]
            yTs = [P.sb("yTsA%d" % i, [128, 256], BF16) for i in range(2)]
            b_yTs = [Buf() for _ in range(2)]
            tile_n = 0
            for g in range(NG if stopA == 0 else 0):
                nb = 2 + g % 2
                P.op("pe", lambda e, nb=nb: e.matmul(bank[nb][:, 0:260], lhsT=zeros[:, 0:128], rhs=zeros[:, 0:260],
                                                    start=True, stop=False, skip_group_check=True),
                     reads=[b_zero], writes=[b_bank[nb]])
                for c in range(4 * g + 4):
                    r = c - 4 * g
                    qlo = 0 if r <= 0 else 128 * r
                    sb_ = tile_n % 2
                    pb = tile_n % NPT
                    tile_n += 1
                    delta = 4 * g - c
                    P.op("pe", lambda e, sb_=sb_, c=c, g=g, qlo=qlo: e.matmul(
                        bank[sb_][:, qlo:512], lhsT=KE[:, c * 128:(c + 1) * 128],
                        rhs=QM[:, g * 512 + qlo:(g + 1) * 512], start=True, stop=True),
                        reads=[b_KE, b_QM], writes=[b_bank[sb_]])
                    P.op("act", lambda e, sb_=sb_, pb=pb, qlo=qlo, delta=delta: e.activation(
                        out=PT[pb][:, qlo:512], in_=bank[sb_][:, qlo:512], func=AF.Exp,
                        bias=btab[:, delta + 3:delta + 4], scale=1.0),
                        reads=[b_bank[sb_], b_btab], writes=[b_PT[pb]])
                    if r >= 0:
                        P.op("pool", lambda e, pb=pb, qlo=qlo: e.tensor_tensor(
                            out=PT[pb][:, qlo:qlo + 128], in0=PT[pb][:, qlo:qlo + 128], in1=tri[:], op=ALU.mult),
                            reads=[b_PT[pb], b_tri], writes=[b_PT[pb]])
                    for sub in range(qlo // 128, 4):
                        last = (c == 4 * g + sub)
                        P.op("pe", lambda e, nb=nb, pb=pb, sub=sub, c=c, last=last: e.matmul(
                            bank[nb][:, sub * 65:(sub + 1) * 65], lhsT=PT[pb][:, sub * 128:(sub + 1) * 128],
                            rhs=VA[:, c, :], start=False, stop=last, skip_group_check=True),
                            reads=[b_PT[pb], b_VA], writes=[b_bank[nb]])
                yb_ = g % 2
                ndv = bank[nb][:, 0:260].rearrange("p (s e) -> p s e", e=65)
                P.op("dve", lambda e, yb_=yb_, ndv=ndv: e.reciprocal(out=rec[yb_][:], in_=ndv[:, :, 64]),
                     reads=[b_bank[nb]], writes=[b_rec[yb_]])
                for sub in range(4):
                    P.op("dve", lambda e, yb_=yb_, sub=sub, nb=nb, g=g: e.scalar_tensor_tensor(
                        out=ya[yb_][:, sub, :], in0=bank[nb][:, sub * 65:sub * 65 + 64], scalar=rec[yb_][:, sub:sub + 1],
                        in1=sgA[:, 4 * g + sub, :], op0=ALU.mult, op1=ALU.mult),
                        reads=[b_bank[nb], b_rec[yb_], b_sgA], writes=[b_ya[yb_]])
                for pr in range(2):
                    P.op("pe", lambda e, yb_=yb_, pr=pr: e.transpose(
                        out=bankT[:, pr * 128:(pr + 1) * 128],
                        in_=ya[yb_][:, 2 * pr:2 * pr + 2, :].rearrange("p s e -> p (s e)"), identity=ident[:]),
                        reads=[b_ya[yb_], b_id], writes=[b_bankT])
                P.op("act", lambda e, yb_=yb_: e.copy(out=yTs[yb_][:], in_=bankT[:, 0:256]),
                     reads=[b_bankT], writes=[b_yTs[yb_]])
                dst = yT_d[0:64, g * 512:(g + 1) * 512].rearrange("e (p s q) -> s e p q", p=2, s=2)
                for s2 in range(2):
                    P.dma(dst[s2], yTs[yb_][s2 * 64:(s2 + 1) * 64, :].rearrange("e (p q) -> e p q", p=2),
                          reads=[b_yTs[yb_]], writes=[b_y])

        if do_bc:
            NT2 = S // 256
            QT, b_QT, KT, b_KT = QM, b_QM, KE, b_KE
            VC, b_VC, sgB, b_sgB = VA, b_VA, sgA, b_sgA
            wT = Wb
            load_w(wT, b_Wb, wT_d, 512)
            wR = P.sb("wR_sb", [128, 8, 322], BF16); b_wR = Buf()
            load_w(wR, b_wR, wR_d, 322)
            cw = P.sb("cw_sb", [64, 10], F32); b_cw = Buf()
            P.dma(cw[:], cw_d, writes=[b_cw])
            misc = P.sb("misc_sb", [128, 8], F32); b_misc = Buf()
            P.dma(misc[:], misc_d, writes=[b_misc])
            wcol = P.sb("wcol_sb", [128, 1], F32); b_wcol = Buf()
            P.dma(wcol[:], wcol_d, writes=[b_wcol])
            trif = P.sb("trif_sb", [128, 128], F32); b_trif = Buf()
            P.dma(trif[:], trif_d, writes=[b_trif])
            identf = P.sb("identf_sb", [128, 128], F32); b_idf = Buf()
            P.dma(identf[:], idf_d, writes=[b_idf])
            onesf = P.sb("onesf_sb", [128, 128], F32); b_ones = Buf()
            P.op("dve", lambda e: e.memset(onesf[:], 1.0), writes=[b_ones])
            VB = P.sb("VB", [128, NCH, 64], BF16); b_VB = Buf()
            gst = [P.sb("gst%d" % i, [128, 2, 128], BF16) for i in range(2)]
            b_gst = [Buf() for _ in range(2)]
            b_gCd = Buf()
            G = P.sb("G", [128, 2, NCH], F32); b_G = Buf()
            TB = [P.sb("TB%d" % i, [128, 4, 256], F32) for i in range(1)] * 2
            b_TB = [Buf()] * 2
            tmpB = [P.sb("tmpB%d" % i, [128, 256], F32) for i in range(2)]
            b_tmpB = [Buf() for _ in range(2)]
            cst = [[P.sb("cst%d_%d" % (a, i), [64, 260], F32) for i in range(2)] for a in range(2)]
            b_cst = [[Buf() for _ in range(2)] for _ in range(2)]
            acc = [P.sb("acc%d" % a, [64, 256], F32) for a in range(2)]
            b_acc = [Buf() for _ in range(2)]
            sgk = P.sb("sgk", [64, 256], F32); b_sgk = Buf()
            for a in range(2):
                P.op("dve", lambda e, a=a: e.memset(cst[a][0][:, 0:3], 0.0), writes=[b_cst[a][0]])
            tabs_v = tabs_d.rearrange("k d t -> d k t")

            for i in range(NT2):
                hb = i % 2
                tb = i % 2
                tsl = slice(i * 256, (i + 1) * 256)
                P.dma(hTt[hb][:, :, 0:256], hT_v[:, :, tsl], writes=[b_hTt[hb]])
                P.dma(TB[tb][hi, :, :], tabs_v[:, :, tsl], writes=[b_TB[tb]])
                for grp in range(4):
                    for mc in range(8):
                        P.op("pe", lambda e, mc=mc, hb=hb, grp=grp: e.matmul(
                            bank[grp][:, 0:256], lhsT=wT[:, mc, grp * 128:(grp + 1) * 128], rhs=hTt[hb][:, mc, 0:256],
                            start=(mc == 0), stop=(mc == 7)),
                            reads=[b_Wb, b_hTt[hb]], writes=[b_bank[grp]])
                for (dst, b_dst, g0, g1, t0) in ((QT, b_QT, 0, 2, 0), (KT, b_KT, 1, 3, 2)):
                    P.op("dve", lambda e, g0=g0, tb=tb, t0=t0: e.tensor_tensor(
                        out=tmpB[0][hi, :], in0=bank[g0][hi, 0:256], in1=TB[tb][hi, t0, :], op=ALU.mult),
                        reads=[b_bank[g0], b_TB[tb]], writes=[b_tmpB[0]])
                    P.op("dve", lambda e, g1=g1, tb=tb, t0=t0: e.tensor_tensor(
                        out=tmpB[1][hi, :], in0=bank[g1][hi, 0:256], in1=TB[tb][hi, t0 + 1, :], op=ALU.mult),
                        reads=[b_bank[g1], b_TB[tb]], writes=[b_tmpB[1]])
                    P.op("pool", lambda e, dst=dst, tsl=tsl: e.tensor_tensor(
                        out=dst[hi, tsl], in0=tmpB[0][hi, :], in1=tmpB[1][hi, :], op=ALU.add),
                        reads=[b_tmpB[0], b_tmpB[1]], writes=[b_dst])
                cb = i % 2
                for a in range(2):
                    P.op("act", lambda e, a=a, cb=cb: e.copy(out=cst[a][cb][:, 3:259], in_=bank[a][lo, 0:256]),
                         reads=[b_bank[a]], writes=[b_cst[a][cb]])
                    if i > 0:
                        P.op("pool", lambda e, a=a, cb=cb: e.tensor_copy(out=cst[a][cb][:, 0:3],
                                                                        in_=cst[a][1 - cb][:, 256:259]),
                             reads=[b_cst[a][1 - cb]], writes=[b_cst[a][cb]])
                    P.op("pool", lambda e, a=a, cb=cb: e.tensor_scalar(
                        out=acc[a][:], in0=cst[a][cb][:, 0:256], scalar1=cw[:, 4 * a:4 * a + 1],
                        scalar2=cw[:, 8 + a:9 + a], op0=ALU.mult, op1=ALU.add),
                        reads=[b_cst[a][cb], b_cw], writes=[b_acc[a]])
                    for jj in range(1, 4):
                        P.op("dve", lambda e, a=a, cb=cb, jj=jj: e.scalar_tensor_tensor(
                            out=acc[a][:], in0=cst[a][cb][:, jj:jj + 256], scalar=cw[:, 4 * a + jj:4 * a + jj + 1],
                            in1=acc[a][:], op0=ALU.mult, op1=ALU.add),
                            reads=[b_cst[a][cb], b_cw, b_acc[a]], writes=[b_acc[a]])
                P.op("act", lambda e, tsl=tsl: e.activation(out=QT[lo, tsl], in_=acc[0][:], func=AF.Silu),
                     reads=[b_acc[0]], writes=[b_QT])
                P.op("act", lambda e: e.activation(out=sgk[:], in_=acc[1][:], func=AF.Sigmoid),
                     reads=[b_acc[1]], writes=[b_sgk])
                P.op("dve", lambda e, tsl=tsl: e.scalar_tensor_tensor(
                    out=KT[lo, tsl], in0=acc[1][:], scalar=0.125, in1=sgk[:], op0=ALU.mult, op1=ALU.mult),
                    reads=[b_acc[1], b_sgk], writes=[b_KT])
                for sub in range(2):
                    ch = 2 * i + sub
                    rb = 4 + sub % 2
                    for mc in range(8):
                        P.op("pe", lambda e, mc=mc, hb=hb, sub=sub, rb=rb: e.matmul(
                            bank[rb][:, 0:322], lhsT=hTt[hb][:, mc, sub * 128:(sub + 1) * 128],
                            rhs=wR[:, mc, :], start=(mc == 0), stop=(mc == 7)),
                            reads=[b_wR, b_hTt[hb]], writes=[b_bank[rb]])
                    P.op("dve", lambda e, ch=ch, rb=rb: e.tensor_copy(out=VB[:, ch, :], in_=bank[rb][:, 0:64]),
                         reads=[b_bank[rb]], writes=[b_VB])
                    P.op("act", lambda e, ch=ch, rb=rb: e.activation(out=sgB[:, ch, :], in_=bank[rb][:, 64:128],
                                                                       func=AF.Silu),
                         reads=[b_bank[rb]], writes=[b_sgB])
                    P.op("dve", lambda e, ch=ch, rb=rb: e.tensor_copy(out=VC[:, ch, 0:64], in_=bank[rb][:, 128:192]),
                         reads=[b_bank[rb]], writes=[b_VC])
                    P.op("act", lambda e, sub=sub, tb=tb, rb=rb: e.activation(out=gst[tb][:, sub, 0:64],
                                                                              in_=bank[rb][:, 192:256], func=AF.Sigmoid),
                         reads=[b_bank[rb]], writes=[b_gst[tb]])
                    P.op("act", lambda e, sub=sub, tb=tb, rb=rb: e.activation(out=gst[tb][:, sub, 64:128],
                                                                              in_=bank[rb][:, 256:320], func=AF.Silu),
                         reads=[b_bank[rb]], writes=[b_gst[tb]])
                    P.op("dve", lambda e, ch=ch, rb=rb: e.tensor_copy(out=G[:, :, ch], in_=bank[rb][:, 320:322]),
                         reads=[b_bank[rb]], writes=[b_G])
                P.dma(gC_d[2 * i:2 * i + 2].rearrange("c p e -> p c e"), gst[tb][:], reads=[b_gst[tb]], writes=[b_gCd])

            cz = P.sb("cz", [128, 10, NCH], F32); b_cz = [Buf() for _ in range(10)]
            rz = P.sb("rz", [128, 6, NCH + 1], F32); b_rz = [Buf() for _ in range(6)]
            Acol = P.sb("Acol", [128, 1], F32); b_Acol = Buf()
            Abc = P.sb("Abc", [128, 128], F32); b_Abc = Buf()
            P.op("dve", lambda e: e.memset(Abc[:], 0.0), writes=[b_Abc])
            P.op("dve", lambda e: e.tensor_scalar(out=cz[:, 0, :], in0=G[:, 1, :], scalar1=misc[:, 2:3], scalar2=None,
                                                  op0=ALU.add), reads=[b_G, b_misc], writes=[b_cz[0]])
            P.op("act", lambda e: e.activation(out=cz[:, 1, :], in_=cz[:, 0, :], func=AF.Exp, scale=-1.0),
                 reads=[b_cz[0]], writes=[b_cz[1]])
            P.op("act", lambda e: e.activation(out=cz[:, 2, :], in_=cz[:, 1, :], func=AF.Ln, bias=1.0),
                 reads=[b_cz[1]], writes=[b_cz[2]])
            P.op("pe", lambda e: e.matmul(bank[0][:, 0:NCH], lhsT=trif[:], rhs=cz[:, 2, :], start=True, stop=True),
                 reads=[b_trif, b_cz[2]], writes=[b_bank[0]])
            P.op("dve", lambda e: e.tensor_copy(out=cz[:, 3, :], in_=bank[0][:, 0:NCH]),
                 reads=[b_bank[0]], writes=[b_cz[3]])
            P.op("dve", lambda e: e.scalar_tensor_tensor(out=cz[:, 4, :], in0=G[:, 0, :], scalar=misc[:, 1:2],
                                                         in1=cz[:, 3, :], op0=ALU.add, op1=ALU.add),
                 reads=[b_G, b_misc, b_cz[3]], writes=[b_cz[4]])
            P.op("pe", lambda e: e.transpose(out=bank[1][0:NCH, 0:128], in_=cz[:, 4, :], identity=identf[:]),
                 reads=[b_cz[4], b_idf], writes=[b_bank[1]])
            P.op("dve", lambda e: e.tensor_reduce(out=Acol[0:NCH, :], in_=bank[1][0:NCH, 0:128], axis=AX.X, op=ALU.max),
                 reads=[b_bank[1]], writes=[b_Acol])
            P.op("dve", lambda e: e.tensor_scalar(out=Abc[0:NCH, :], in0=Abc[0:NCH, :], scalar1=0.0,
                                                  scalar2=Acol[0:NCH, 0:1], op0=ALU.mult, op1=ALU.add),
                 reads=[b_Acol, b_Abc], writes=[b_Abc])
            P.op("pe", lambda e: e.transpose(out=bank[2][:, 0:128], in_=Abc[:, :], identity=identf[:]),
                 reads=[b_Abc, b_idf], writes=[b_bank[2]])
            P.op("dve", lambda e: e.tensor_copy(out=rz[:, 0, 0:NCH], in_=bank[2][:, 0:NCH]),
                 reads=[b_bank[2]], writes=[b_rz[0]])
            P.op("pe", lambda e: e.matmul(bank[3][:, 0:NCH], lhsT=onesf[:], rhs=cz[:, 2, :], start=True, stop=True),
                 reads=[b_ones, b_cz[2]], writes=[b_bank[3]])
            P.op("dve", lambda e: e.tensor_scalar(out=rz[:, 1, 0:NCH], in0=bank[3][:, 0:NCH], scalar1=-1.0,
                                                  scalar2=None, op0=ALU.mult),
                 reads=[b_bank[3]], writes=[b_rz[1]])
            P.op("dve", lambda e: e.memset(rz[:, 2, 0:1], 0.0), writes=[b_rz[2]])
            P.op("dve", lambda e: e.tensor_tensor_scan(out=rz[:, 2, 1:NCH + 1], data0=rz[:, 0, 0:NCH],
                                                       data1=rz[:, 1, 0:NCH], initial=0.0, op0=ALU.max, op1=ALU.add),
                 reads=[b_rz[0], b_rz[1], b_rz[2]], writes=[b_rz[2]])
            P.op("dve", lambda e: e.tensor_tensor(out=rz[:, 3, 0:NCH], in0=rz[:, 2, 0:NCH], in1=rz[:, 0, 0:NCH],
                                                  op=ALU.max),
                 reads=[b_rz[2], b_rz[0]], writes=[b_rz[3]])
            P.op("dve", lambda e: e.tensor_tensor(out=rz[:, 4, 0:NCH], in0=rz[:, 2, 0:NCH], in1=rz[:, 3, 0:NCH],
                                                  op=ALU.subtract),
                 reads=[b_rz[2], b_rz[3]], writes=[b_rz[4]])
            P.op("act", lambda e: e.activation(out=rz[:, 5, 0:NCH], in_=rz[:, 4, 0:NCH], func=AF.Exp),
                 reads=[b_rz[4]], writes=[b_rz[5]])
            P.op("dve", lambda e: e.tensor_tensor(out=cz[:, 5, :], in0=cz[:, 4, :], in1=rz[:, 3, 0:NCH], op=ALU.subtract),
                 reads=[b_cz[4], b_rz[3]], writes=[b_cz[5]])
            P.op("act", lambda e: e.activation(out=cz[:, 6, :], in_=cz[:, 5, :], func=AF.Exp),
                 reads=[b_cz[5]], writes=[b_cz[6]])
            P.op("dve", lambda e: e.tensor_tensor(out=cz[:, 7, :], in0=cz[:, 3, :], in1=rz[:, 3, 0:NCH], op=ALU.subtract),
                 reads=[b_cz[3], b_rz[3]], writes=[b_cz[7]])
            P.op("act", lambda e: e.activation(out=cz[:, 8, :], in_=cz[:, 7, :], func=AF.Exp),
                 reads=[b_cz[7]], writes=[b_cz[8]])
            for ch in range(NCH):
                P.op("pool", lambda e, ch=ch: e.tensor_scalar(out=VC[:, ch, :], in0=VC[:, ch, :],
                                                               scalar1=cz[:, 6, ch:ch + 1], scalar2=None, op0=ALU.mult),
                     reads=[b_VC, b_cz[6]], writes=[b_VC])

            Krow = [P.sb("Krow%d" % i, [128, 128], BF16) for i in range(2)]
            b_Krow = [Buf() for _ in range(2)]
            qbt = [P.sb("qbt%d" % i, [128, 128], BF16) for i in range(2)]
            qct = [P.sb("qct%d" % i, [128, 128], BF16) for i in range(2)]
            b_qbt = [Buf() for _ in range(2)]
            b_qct = [Buf() for _ in range(2)]
            Cbf = [P.sb("Cbf%d" % i, [128, 65], BF16) for i in range(2)]
            Rbf = [P.sb("Rbf%d" % i, [128, 64], BF16) for i in range(2)]
            b_Cbf = [Buf() for _ in range(2)]
            b_Rbf = [Buf() for _ in range(2)]
            Cst = [P.sb("Cst%d" % i, [64, 65], F32) for i in range(2)]
            b_Cst = [Buf() for _ in range(2)]
            Rst = [P.sb("Rst%d" % i, [128, 64], F32) for i in range(2)]
            b_Rst = [Buf() for _ in range(2)]
            kvc = P.sb("kvc", [64, 65], F32); b_kvc = Buf()
            kvb = P.sb("kvb", [128, 64], F32); b_kvb = Buf()
            for i in range(2):
                P.op("dve", lambda e, i=i: e.memset(qbt[i][:], 0.0), writes=[b_qbt[i]])
                P.op("dve", lambda e, i=i: e.memset(qct[i][:], 0.0), writes=[b_qct[i]])
                P.op("dve", lambda e, i=i: e.memset(Cbf[i][:], 0.0), writes=[b_Cbf[i]])
                P.op("dve", lambda e, i=i: e.memset(Rbf[i][:], 0.0), writes=[b_Rbf[i]])
            P.op("dve", lambda e: e.memset(Cst[0][:], 0.0), writes=[b_Cst[0]])
            P.op("dve", lambda e: e.memset(Rst[0][:], 0.0), writes=[b_Rst[0]])
            STm = [P.sb("STm%d" % i, [128, 128], BF16) for i in range(4)]
            b_STm = [Buf() for _ in range(4)]
            st6 = [P.sb("st6_%d" % i, [128, 6], F32) for i in range(2)]
            mv = [P.sb("mv%d" % i, [128, 4], F32) for i in range(2)]
            t1 = [P.sb("t1_%d" % i, [128, 64], F32) for i in range(2)]
            hs = P.sb("hs", [128, 64], F32); b_hs = Buf()
            dm = P.sb("dm", [128, 2], F32); b_dm = Buf()
            yn = [P.sb("yn%d" % i, [128, 128], BF16) for i in range(2)]
            b_st6 = [Buf() for _ in range(2)]
            b_mv = [Buf() for _ in range(2)]
            b_t1 = [Buf() for _ in range(2)]
            b_yn = [Buf() for _ in range(2)]
            yTsBC = [P.sb("yTsBC%d" % i, [128, 512], BF16) for i in range(1)] * 2
            b_yTsBC = [Buf()] * 2
            gct = [P.sb("gct%d" % i, [128, 128], BF16) for i in range(2)]
            b_gct = [Buf() for _ in range(2)]
            stm_n = [0]

            def head_norm(src_ap, b_src, gate_ap, b_gate, k, ynk, col0):
                P.op("dve", lambda e: e.bn_stats(out=st6[k][:], in_=src_ap), reads=[b_src], writes=[b_st6[k]])
                P.op("dve", lambda e: e.bn_aggr(out=mv[k][:, 0:2], in_=st6[k][:]), reads=[b_st6[k]], writes=[b_mv[k]])
                P.op("dve", lambda e: e.tensor_scalar(out=mv[k][:, 2:3], in0=mv[k][:, 1:2], scalar1=1e-5, scalar2=None,
                                                      op0=ALU.add), reads=[b_mv[k]], writes=[b_mv[k]])
                P.op("act", lambda e: e.activation(out=mv[k][:, 3:4], in_=mv[k][:, 2:3], func=AF.Sqrt),
                     reads=[b_mv[k]], writes=[b_mv[k]])
                P.op("dve", lambda e: e.reciprocal(out=mv[k][:, 2:3], in_=mv[k][:, 3:4]),
                     reads=[b_mv[k]], writes=[b_mv[k]])
                P.op("dve", lambda e: e.scalar_tensor_tensor(out=t1[k][:], in0=src_ap, scalar=mv[k][:, 0:1],
                                                             in1=gate_ap, op0=ALU.subtract, op1=ALU.mult),
                     reads=[b_src, b_mv[k], b_gate], writes=[b_t1[k]])
                P.op("dve", lambda e: e.tensor_scalar(out=yn[ynk][:, col0:col0 + 64], in0=t1[k][:], scalar1=mv[k][:, 2:3],
                                                      scalar2=None, op0=ALU.mult),
                     reads=[b_t1[k], b_mv[k]], writes=[b_yn[ynk]])

            for ch in range(NCH):
                kb = ch % 2
                csl = slice(ch * 128, (ch + 1) * 128)
                cbk, nt = ch // 2, ch % 2
                P.dma(gct[kb][:], gC_d[ch], reads=[b_gCd], writes=[b_gct[kb]])
                P.op("pool", lambda e, kb=kb, csl=csl: e.tensor_copy(out=qbt[kb][hi, :], in_=QT[hi, csl]),
                     reads=[b_QT], writes=[b_qbt[kb]])
                P.op("pool", lambda e, kb=kb, csl=csl: e.tensor_copy(out=qct[kb][lo, :], in_=QT[lo, csl]),
                     reads=[b_QT], writes=[b_qct[kb]])
                si = stm_n[0] % 4
                stm_n[0] += 1
                pb_ = si % 2
                P.op("pe", lambda e, pb_=pb_, csl=csl, kb=kb: e.matmul(bank[pb_][:, 0:128], lhsT=KT[:, csl], rhs=qct[kb][:],
                                                                      start=True, stop=True),
                     reads=[b_KT, b_qct[kb]], writes=[b_bank[pb_]])
                P.op("dve", lambda e, si=si, pb_=pb_: e.tensor_tensor(out=STm[si][:], in0=bank[pb_][:, 0:128], in1=tri[:],
                                                                    op=ALU.mult),
                     reads=[b_bank[pb_], b_tri], writes=[b_STm[si]])
                P.op("pe", lambda e, kb=kb: e.matmul(bank[2][:, 0:65], lhsT=qct[kb][:], rhs=Cbf[kb][:],
                                                    start=True, stop=False),
                     reads=[b_qct[kb], b_Cbf[kb]], writes=[b_bank[2]])
                P.op("pe", lambda e, si=si, ch=ch: e.matmul(bank[2][:, 0:65], lhsT=STm[si][:], rhs=VC[:, ch, :],
                                                           start=False, stop=True),
                     reads=[b_STm[si], b_VC], writes=[b_bank[2]])
                P.op("dve", lambda e: e.tensor_scalar(out=dm[:, 1:2], in0=bank[2][:, 64:65], scalar1=-1.0, scalar2=None,
                                                      op0=ALU.mult), reads=[b_bank[2]], writes=[b_dm])
                P.op("dve", lambda e, ch=ch: e.scalar_tensor_tensor(
                    out=dm[:, 0:1], in0=bank[2][:, 64:65], scalar=cz[:, 8, ch:ch + 1], in1=dm[:, 1:2],
                    op0=ALU.max, op1=ALU.max), reads=[b_bank[2], b_cz[8], b_dm], writes=[b_dm])
                P.op("dve", lambda e: e.reciprocal(out=dm[:, 1:2], in_=dm[:, 0:1]), reads=[b_dm], writes=[b_dm])
                P.op("dve", lambda e, ch=ch, kb=kb: e.scalar_tensor_tensor(
                    out=hs[:], in0=bank[2][:, 0:64], scalar=dm[:, 1:2], in1=gct[kb][:, 0:64], op0=ALU.mult, op1=ALU.mult),
                    reads=[b_bank[2], b_dm, b_gct[kb]], writes=[b_hs])
                head_norm(hs[:], b_hs, gct[kb][:, 64:128], b_gct[kb], 1, kb, 64)
                used = []
                for mt in range(nt + 1):
                    tk = 2 * cbk + mt
                    ksl = slice(tk * 128, (tk + 1) * 128)
                    si = stm_n[0] % 4
                    stm_n[0] += 1
                    pb_ = si % 2
                    P.op("pe", lambda e, pb_=pb_, ksl=ksl, kb=kb: e.matmul(
                        bank[pb_][:, 0:128], lhsT=KT[:, ksl], rhs=qbt[kb][:], start=True, stop=True),
                        reads=[b_KT, b_qbt[kb]], writes=[b_bank[pb_]])
                    if mt == nt:
                        P.op("dve", lambda e, si=si, pb_=pb_: e.tensor_tensor(out=STm[si][:], in0=bank[pb_][:, 0:128],
                                                                            in1=tri[:], op=ALU.mult),
                             reads=[b_bank[pb_], b_tri], writes=[b_STm[si]])
                    else:
                        P.op("act", lambda e, si=si, pb_=pb_: e.copy(out=STm[si][:], in_=bank[pb_][:, 0:128]),
                             reads=[b_bank[pb_]], writes=[b_STm[si]])
                    used.append((si, tk))
                rk = cbk % 2
                P.op("pe", lambda e, kb=kb, rk=rk: e.matmul(bank[3][:, 0:64], lhsT=qbt[kb][:], rhs=Rbf[rk][:],
                                                           start=True, stop=False),
                     reads=[b_qbt[kb], b_Rbf[rk]], writes=[b_bank[3]])
                for n_, (si, tk) in enumerate(used):
                    P.op("pe", lambda e, si=si, tk=tk, last=(n_ == len(used) - 1): e.matmul(
                        bank[3][:, 0:64], lhsT=STm[si][:], rhs=VB[:, tk, :], start=False, stop=last),
                        reads=[b_STm[si], b_VB], writes=[b_bank[3]])
                head_norm(bank[3][:, 0:64], b_bank[3], sgB[:, ch, :], b_sgB, 0, kb, 0)
                sub = ch % 4
                yb_ = (ch // 4) % 2
                P.op("pe", lambda e, kb=kb, sub=sub: e.transpose(out=bankT[:, sub * 128:(sub + 1) * 128], in_=yn[kb][:],
                                                                identity=ident[:]),
                     reads=[b_yn[kb], b_id], writes=[b_bankT])
                P.op("dve", lambda e, yb_=yb_, sub=sub: e.tensor_scalar(
                    out=yTsBC[yb_][:, sub * 128:(sub + 1) * 128], in0=bankT[:, sub * 128:(sub + 1) * 128],
                    scalar1=wcol[:, 0:1], scalar2=None, op0=ALU.mult),
                    reads=[b_bankT, b_wcol], writes=[b_yTsBC[yb_]])
                if sub == 3:
                    g = ch // 4
                    P.dma(yT_d[64:192, g * 512:(g + 1) * 512], yTsBC[yb_][:], reads=[b_yTsBC[yb_]], writes=[b_y])
                tcol = 512 + (ch % 4) * 128
                P.op("pe", lambda e, tcol=tcol, csl=csl: e.transpose(out=bankT[:, tcol:tcol + 128], in_=KT[:, csl],
                                                                   identity=ident[:]),
                     reads=[b_KT, b_id], writes=[b_bankT])
                P.op("act", lambda e, kb=kb, tcol=tcol: e.copy(out=Krow[kb][:], in_=bankT[:, tcol:tcol + 128]),
                     reads=[b_bankT], writes=[b_Krow[kb]])
                if ch + 1 < NCH:
                    P.op("pe", lambda e, kb=kb, ch=ch: e.matmul(bank[4][:, 0:65], lhsT=Krow[kb][:], rhs=VC[:, ch, :],
                                                               start=True, stop=True),
                         reads=[b_Krow[kb], b_VC], writes=[b_bank[4]])
                    o_, n2 = ch % 2, (ch + 1) % 2
                    P.op("dve", lambda e, ch=ch: e.tensor_scalar(out=kvc[:], in0=bank[4][lo, 0:65],
                                                                 scalar1=rz[lo, 5, ch + 1:ch + 2], scalar2=None,
                                                                 op0=ALU.mult),
                         reads=[b_bank[4], b_rz[5]], writes=[b_kvc])
                    P.op("dve", lambda e, ch=ch, o_=o_, n2=n2: e.scalar_tensor_tensor(
                        out=Cst[n2][:], in0=Cst[o_][:], scalar=rz[lo, 5, ch + 1:ch + 2], in1=kvc[:],
                        op0=ALU.mult, op1=ALU.add),
                        reads=[b_Cst[o_], b_rz[5], b_kvc], writes=[b_Cst[n2]])
                    P.op("pool", lambda e, n2=n2: e.tensor_copy(out=Cbf[n2][lo, :], in_=Cst[n2][:]),
                         reads=[b_Cst[n2]], writes=[b_Cbf[n2]])
                if cbk + 1 < NCH // 2:
                    P.op("pe", lambda e, kb=kb, ch=ch, nt=nt: e.matmul(bank[5][:, 0:64], lhsT=Krow[kb][:], rhs=VB[:, ch, :],
                                                                      start=(nt == 0), stop=(nt == 1)),
                         reads=[b_Krow[kb], b_VB], writes=[b_bank[5]])
                    if nt == 1:
                        o_, n2 = cbk % 2, (cbk + 1) % 2
                        P.op("dve", lambda e: e.tensor_scalar(out=kvb[hi, :], in0=bank[5][hi, 0:64], scalar1=misc[hi, 0:1],
                                                              scalar2=None, op0=ALU.mult),
                             reads=[b_bank[5], b_misc], writes=[b_kvb])
                        P.op("dve", lambda e, o_=o_, n2=n2: e.scalar_tensor_tensor(
                            out=Rst[n2][hi, :], in0=Rst[o_][hi, :], scalar=misc[hi, 0:1], in1=kvb[hi, :],
                            op0=ALU.mult, op1=ALU.add),
                            reads=[b_Rst[o_], b_misc, b_kvb], writes=[b_Rst[n2]])
                        P.op("pool", lambda e, n2=n2: e.tensor_copy(out=Rbf[n2][hi, :], in_=Rst[n2][hi, :]),
                             reads=[b_Rst[n2]], writes=[b_Rbf[n2]])

        P.finish([b_y])
    return nc


def alibi_slope(h):
    return float(2.0 ** (-8.0 * (h + 1) / 8))


def pb_consts(c, S=S):
    slope = alibi_slope(c)
    p = np.arange(128, dtype=np.float64)
    E = np.zeros((64, S), np.float32)
    E[np.arange(S) // 256, np.arange(S)] = 1.0
    tri = (p[:, None] <= p[None, :]).astype(np.float32)
    caus = np.zeros((128, 128), np.float32)
    caus[:, 64:] = -1e30
    deltas = np.arange(-3, 125, dtype=np.float64)
    btab = slope * (p[:, None] - 128.0 * deltas[None, :])
    negb = -BIG - slope * (128.0 * np.arange(4)[None, :] + p[:, None])
    return {
        "ident": _ident(), "Emat": E.astype(NPBF), "tri": tri.astype(NPBF), "caus": caus,
        "btab": btab.astype(np.float32), "negb": negb.astype(np.float32),
    }


def pb_consts_bc(c, S=S):
    hb = c % 4
    gamma = 1.0 - 2.0 ** (-5.0 - hb)
    L = 256
    theta = (1.0 / (10000.0 ** np.linspace(0.0, 1.0, 32, dtype=np.float32))).astype(np.float32)
    pos = np.arange(S, dtype=np.float32)
    ang = pos[None, :] * np.concatenate([theta, theta])[:, None]
    cos, sin = np.cos(ang).astype(np.float64), np.sin(ang).astype(np.float64)
    sgn = np.concatenate([-np.ones(32), np.ones(32)])[:, None]
    n = (np.arange(S) % L).astype(np.float64)[None, :]
    gq = gamma ** n
    gk = gamma ** (-n) / 8.0
    tabs = np.stack([cos * gq, sgn * sin * gq, cos * gk, sgn * sin * gk]).astype(np.float32)
    p = np.arange(128)
    trif = (p[:, None] <= p[None, :]).astype(np.float32)
    return {"tabs": tabs, "trif": trif, "identf": np.eye(128, dtype=np.float32), "gammaL": np.float32(gamma ** L)}


def pb_inputs(c, hT, w_in_l, conv_w_l, conv_b_l, gate_bias_l, ret_w_l, ml_w_l, S=S, do_a=True, do_bc=True):
    m = {"hT": hT}
    m.update(pb_consts(c, S=S))
    r = np.arange(64)
    if do_a:
        cols = np.concatenate([c * 64 + r + off for off in (0, 512, 512, 0, 1024, 1536)])
        m["wA"] = np.ascontiguousarray(w_in_l[:, cols])
    else:
        m["wA"] = np.zeros((D, 384), np.float32)
    if do_bc:
        h = c % 4
        B0, C0 = 2048, 3072
        bq, bk, bv, bg = [B0 + k * 256 + h * 64 + r for k in range(4)]
        cq, ck, cv, co, cg = [C0 + k * 256 + h * 64 + r for k in range(5)]
        ci = np.array([C0 + 5 * 256 + h]); cf = np.array([C0 + 5 * 256 + 4 + h])
        sw = np.concatenate([r[32:], r[:32]])
        colsT = np.concatenate([cq, bq, ck, bk, cq, bq[sw], ck, bk[sw]])
        colsR = np.concatenate([bv, bg, cv, co, cg, ci, cf])
        m["wT"] = np.ascontiguousarray(w_in_l[:, colsT])
        m["wR"] = np.ascontiguousarray(w_in_l[:, colsR])
        cb = pb_consts_bc(c, S=S)
        gl = cb.pop("gammaL")
        m.update(cb)
        cw = np.zeros((64, 10), np.float32)
        cw[:, 0:4] = conv_w_l[:, h * 64 + r].T
        cw[:, 4:8] = conv_w_l[:, 256 + h * 64 + r].T
        cw[:, 8] = conv_b_l[h * 64 + r]
        cw[:, 9] = conv_b_l[256 + h * 64 + r]
        m["cw"] = cw
        misc = np.zeros((128, 8), np.float32)
        misc[:, 0] = gl
        misc[:, 1] = gate_bias_l[h]
        misc[:, 2] = gate_bias_l[4 + h]
        m["misc"] = misc
        m["wcol"] = np.concatenate([ret_w_l[h * 64 + r], ml_w_l[h * 64 + r]])[:, None].astype(np.float32)
    return m


_PROGS = {}


def _prog(name, fn):
    if name not in _PROGS:
        _PROGS[name] = fn()
    return _PROGS[name]


def _run(nc, maps):
    return run_bass_kernel_spmd(nc, maps, core_ids=list(range(NCORES))).results


def kernel(x, norm_w, w_in, conv_w, conv_b, gate_bias, ret_norm_w, mlstm_norm_w, w_out, final_norm_w):
    f32 = np.float32
    x2 = np.ascontiguousarray(np.asarray(x, f32)[0])
    norm_w, w_in, conv_w, conv_b = (np.asarray(a, f32) for a in (norm_w, w_in, conv_w, conv_b))
    gate_bias, ret_norm_w, mlstm_norm_w, w_out = (np.asarray(a, f32) for a in (gate_bias, ret_norm_w, mlstm_norm_w, w_out))
    final_norm_w = np.asarray(final_norm_w, f32)
    ident = _ident()
    xs = [x2[c * TOK:(c + 1) * TOK] for c in range(NCORES)]
    res = _run(_prog("pa_first", lambda: build_pa(True, False)),
               [{"x": xs[c], "nw": norm_w[0][None, :], "ident": ident} for c in range(NCORES)])
    hT = np.concatenate([r["hT"] for r in res], axis=1)
    out = None
    for l in range(DEPTH):
        res = _run(_prog("pb", lambda: build_pb(True, True)),
                   [pb_inputs(c, hT, w_in[l], conv_w[l], conv_b[l], gate_bias[l], ret_norm_w[l], mlstm_norm_w[l])
                    for c in range(NCORES)])
        yT = np.concatenate([res[c]["yT"][0:64] for c in range(8)]
                            + [res[h]["yT"][64:128] for h in range(4)]
                            + [res[4 + h]["yT"][128:192] for h in range(4)], axis=0)
        last = l == DEPTH - 1
        nw = final_norm_w if last else norm_w[l + 1]
        maps = [{"x": xs[c], "nw": nw[None, :], "ident": ident,
                 "yT": np.ascontiguousarray(yT[:, c * TOK:(c + 1) * TOK]), "wo": w_out[l]} for c in range(NCORES)]
        if last:
            res = _run(_prog("pa_final", lambda: build_pa(False, True)), maps)
            out = np.concatenate([r["out"] for r in res], axis=0)
        else:
            res = _run(_prog("pa_mid", lambda: build_pa(False, False)), maps)
            xs = [r["xo"] for r in res]
            hT = np.concatenate([r["hT"] for r in res], axis=1)
    return out[None].astype(np.float32)
```

```python
import contextlib
import numpy as np
import ml_dtypes
import concourse.bass as bass
import concourse.mybir as mybir
from concourse.bass_utils import run_bass_kernel_spmd

F32 = mybir.dt.float32
BF16 = mybir.dt.bfloat16
ALU = mybir.AluOpType
AF = mybir.ActivationFunctionType
AX = mybir.AxisListType
NPBF = ml_dtypes.bfloat16

NCORES = 8
S = 16384
D = 1024
DEPTH = 4
TOK = S // NCORES
RMS_EPS = 1e-6


class Buf:
    __slots__ = ("w", "r", "name", "psum")

    def __init__(self, name="", psum=False):
        self.w = None
        self.r = []
        self.name = name
        self.psum = psum


class Prog:
    CENG = ("pe", "act", "dve", "pool")
    QENG = ("sp",)
    NDMA = 24

    def __init__(self, nc, stack):
        self.nc = nc
        self.stack = stack
        self.sem = {e: stack.enter_context(nc.semaphore("s_" + e)) for e in self.CENG}
        self.cnt = {e: 0 for e in self.CENG}
        self.q = {e: [] for e in self.CENG + self.QENG}
        self.seen = {e: {} for e in self.CENG + self.QENG}
        self.needed = {e: set() for e in self.CENG}
        self.dsem = [stack.enter_context(nc.semaphore("s_dma%d" % i)) for i in range(self.NDMA)]
        self.dval = [0] * self.NDMA
        self.drr = 0
        self.nops = 0

    def sb(self, name, shape, dt):
        return self.stack.enter_context(self.nc.sbuf_tensor(name, shape, dt))

    def ps(self, name, shape, dt):
        return self.stack.enter_context(self.nc.psum_tensor(name, shape, dt))

    def _deps(self, reads, writes):
        ev = []
        for b in reads:
            if b.w is not None:
                ev.append(b.w)
        for b in writes:
            if b.w is not None:
                ev.append(b.w)
            ev.extend(b.r)
        return ev

    def _waits(self, eng, evs):
        need = {}
        for (key, val) in evs:
            if key == eng and eng == "pe":
                continue
            if need.get(key, 0) < val:
                need[key] = val
        for key, val in need.items():
            if self.seen[eng].get(key, 0) >= val:
                continue
            self.seen[eng][key] = val
            if key in self.needed:
                self.needed[key].add(val)
            self.q[eng].append(("wait", key, val))

    def _mark(self, ev, reads, writes):
        for b in reads:
            b.r.append(ev)
        for b in writes:
            b.w = ev
            b.r = []
        self.nops += 1

    def op(self, eng, fn, reads=(), writes=()):
        reads = [b for b in reads if b is not None]
        writes = [b for b in writes if b is not None]
        if eng != "pe":
            writes = writes + [b for b in reads if b.psum and b not in writes]
        self._waits(eng, self._deps(reads, writes))
        self.cnt[eng] += 1
        idx = self.cnt[eng]
        self.q[eng].append(("op", fn, idx))
        self._mark((eng, idx), reads, writes)

    def dma(self, out, in_, reads=(), writes=(), q="sp", **kw):
        reads = [b for b in reads if b is not None]
        writes = [b for b in writes if b is not None]
        i = self.drr
        self.drr = (i + 1) % self.NDMA
        evs = self._deps(reads, writes)
        key = "dma%d" % i
        if self.dval[i] > 0:
            evs.append((key, self.dval[i]))
        self._waits(q, evs)
        self.dval[i] += 16
        self.q[q].append(("dma", out, in_, i, kw))
        self._mark((key, self.dval[i]), reads, writes)

    def finish(self, out_bufs):
        evs = [b.w for b in out_bufs if b.w is not None]
        self._waits("sp", evs)
        rank = {}
        for e in self.CENG:
            rank[e] = {idx: k + 1 for k, idx in enumerate(sorted(self.needed[e]))}

        def emit(engname, e):
            for item in self.q[engname]:
                if item[0] == "wait":
                    _, key, val = item
                    if key in rank:
                        e.wait_ge(self.sem[key], rank[key][val])
                    else:
                        e.wait_ge(self.dsem[int(key[3:])], val)
                elif item[0] == "op":
                    _, fn, idx = item
                    ins = fn(e)
                    if idx in rank[engname]:
                        ins.then_inc(self.sem[engname], 1)
                else:
                    _, out, in_, i, kw = item
                    e.dma_start(out=out, in_=in_, **kw).then_inc(self.dsem[i], 16)

        with self.nc.Block() as block:
            @block.sync
            def _(e):
                emit("sp", e)

            @block.tensor
            def _(e):
                emit("pe", e)

            @block.scalar
            def _(e):
                emit("act", e)

            @block.vector
            def _(e):
                emit("dve", e)

            @block.gpsimd
            def _(e):
                emit("pool", e)


def bcast_rows(ap_1xn, nparts):
    t = ap_1xn.tensor
    n = ap_1xn.shape[-1]
    return bass.AP(t, ap_1xn.offset, [[0, nparts], [1, n]])


def build_pa(first, final):
    nc = bass.Bass("TRN2", target_bir_lowering=False)
    NT = TOK // 128
    x_d = nc.dram_tensor("x", [TOK, D], F32, kind="ExternalInput").ap()
    nw_d = nc.dram_tensor("nw", [1, D], F32, kind="ExternalInput").ap()
    id_d = nc.dram_tensor("ident", [128, 128], BF16, kind="ExternalInput").ap()
    if not first:
        yT_d = nc.dram_tensor("yT", [D, TOK], BF16, kind="ExternalInput").ap()
        wo_d = nc.dram_tensor("wo", [D, D], F32, kind="ExternalInput").ap()
        if not final:
            xo_d = nc.dram_tensor("xo", [TOK, D], F32, kind="ExternalOutput").ap()
    if final:
        out_d = nc.dram_tensor("out", [TOK, D], F32, kind="ExternalOutput").ap()
    else:
        hT_d = nc.dram_tensor("hT", [D, TOK], BF16, kind="ExternalOutput").ap()

    with contextlib.ExitStack() as st:
        P = Prog(nc, st)
        nwb = P.sb("nwb", [128, D], F32)
        ident = P.sb("ident_sb", [128, 128], BF16)
        b_nwb, b_id = Buf(), Buf()
        P.dma(nwb[:], bcast_rows(nw_d, 128), writes=[b_nwb])
        P.dma(ident[:], id_d, writes=[b_id])
        NB = 2
        xt = [P.sb("xt%d" % i, [128, D], F32) for i in range(NB)]
        b_xt = [Buf() for _ in range(NB)]
        junk = P.sb("junk", [128, D], F32)
        b_junk = Buf()
        ss = [P.sb("ss%d" % i, [128, 4], F32) for i in range(NB)]
        b_ss = [Buf() for _ in range(NB)]
        if not final:
            hb = [P.sb("hb%d" % i, [128, D], BF16) for i in range(NB)]
            b_hb = [Buf() for _ in range(NB)]
            hTs = P.sb("hTs", [128, 8, TOK], BF16)
            b_hTs = Buf()
            pt = [P.ps("pt%d" % i, [128, 1024], BF16) for i in range(2)]
            b_pt = [Buf(psum=True) for _ in range(2)]
        else:
            ho = [P.sb("ho%d" % i, [128, D], F32) for i in range(NB)]
            b_ho = [Buf() for _ in range(NB)]
        b_out = Buf()
        if not first:
            yTs = P.sb("yTs", [128, 8, TOK], BF16)
            b_yTs = Buf()
            P.dma(yTs[:], yT_d.rearrange("(mc p) t -> p mc t", p=128), writes=[b_yTs])
            wob = P.sb("wob", [128, 8, D], BF16)
            b_wob = Buf()
            wst = [P.sb("wst%d" % i, [128, D], F32) for i in range(2)]
            b_wst = [Buf() for _ in range(2)]
            for mc in range(8):
                k = mc % 2
                P.dma(wst[k][:], wo_d[mc * 128:(mc + 1) * 128, :], writes=[b_wst[k]])
                P.op("pool", lambda e, mc=mc, k=k: e.tensor_copy(out=wob[:, mc, :], in_=wst[k][:]),
                     reads=[b_wst[k]], writes=[b_wob])
            pacc = [P.ps("pacc%d" % i, [128, 512], F32) for i in range(4)]
            b_pacc = [Buf(psum=True) for _ in range(4)]
            b_xo = Buf()

        for tt in range(NT):
            k = tt % NB
            tsl = slice(tt * 128, (tt + 1) * 128)
            P.dma(xt[k][:], x_d[tsl, :], writes=[b_xt[k]])
            if not first:
                for half in range(2):
                    pa_i = (tt * 2 + half) % 4
                    for mc in range(8):
                        P.op("pe", lambda e, mc=mc, half=half, pa_i=pa_i, tsl=tsl: e.matmul(
                            pacc[pa_i][:], lhsT=yTs[:, mc, tsl], rhs=wob[:, mc, half * 512:(half + 1) * 512],
                            start=(mc == 0), stop=(mc == 7)),
                            reads=[b_yTs, b_wob], writes=[b_pacc[pa_i]])
                    P.op("dve", lambda e, k=k, half=half, pa_i=pa_i: e.tensor_tensor(
                        out=xt[k][:, half * 512:(half + 1) * 512], in0=xt[k][:, half * 512:(half + 1) * 512],
                        in1=pacc[pa_i][:], op=ALU.add),
                        reads=[b_pacc[pa_i], b_xt[k]], writes=[b_xt[k]])
                if not final:
                    P.dma(xo_d[tsl, :], xt[k][:], reads=[b_xt[k]], writes=[b_xo])
            P.op("act", lambda e, k=k: e.activation(out=junk[:], in_=xt[k][:], func=AF.Square,
                                                     accum_out=ss[k][:, 0:1]),
                 reads=[b_xt[k]], writes=[b_junk, b_ss[k]])
            P.op("dve", lambda e, k=k: e.tensor_scalar(out=ss[k][:, 1:2], in0=ss[k][:, 0:1], scalar1=1.0 / D,
                                                       scalar2=RMS_EPS, op0=ALU.mult, op1=ALU.add),
                 reads=[b_ss[k]], writes=[b_ss[k]])
            P.op("act", lambda e, k=k: e.activation(out=ss[k][:, 2:3], in_=ss[k][:, 1:2], func=AF.Sqrt),
                 reads=[b_ss[k]], writes=[b_ss[k]])
            P.op("dve", lambda e, k=k: e.reciprocal(out=ss[k][:, 3:4], in_=ss[k][:, 2:3]),
                 reads=[b_ss[k]], writes=[b_ss[k]])
            if final:
                P.op("dve", lambda e, k=k: e.scalar_tensor_tensor(
                    out=ho[k][:], in0=xt[k][:], scalar=ss[k][:, 3:4], in1=nwb[:], op0=ALU.mult, op1=ALU.mult),
                    reads=[b_xt[k], b_ss[k], b_nwb], writes=[b_ho[k]])
                P.dma(out_d[tsl, :], ho[k][:], reads=[b_ho[k]], writes=[b_out])
            else:
                P.op("dve", lambda e, k=k: e.scalar_tensor_tensor(
                    out=hb[k][:], in0=xt[k][:], scalar=ss[k][:, 3:4], in1=nwb[:], op0=ALU.mult, op1=ALU.mult),
                    reads=[b_xt[k], b_ss[k], b_nwb], writes=[b_hb[k]])
                pk = tt % 2
                for mc in range(8):
                    P.op("pe", lambda e, k=k, mc=mc, pk=pk: e.transpose(
                        out=pt[pk][:, mc * 128:(mc + 1) * 128], in_=hb[k][:, mc * 128:(mc + 1) * 128],
                        identity=ident[:]),
                        reads=[b_hb[k], b_id], writes=[b_pt[pk]])
                P.op("act", lambda e, pk=pk, tsl=tsl: e.copy(
                    out=hTs[:, :, tsl], in_=pt[pk][:].rearrange("p (m t) -> p m t", m=8)),
                    reads=[b_pt[pk]], writes=[b_hTs])
        outs = []
        if not final:
            P.dma(hT_d.rearrange("(mc p) t -> p mc t", p=128), hTs[:], reads=[b_hTs], writes=[b_out])
            outs.append(b_out)
            if not first:
                outs.append(b_xo)
        else:
            outs.append(b_out)
        P.finish(outs)
    return nc


def _ident():
    return np.eye(128, dtype=np.float32).astype(NPBF)


BIG = 30000.0
NBLK = S // 256
NCH = S // 128
NG = S // 512


def build_pb(do_a=True, do_bc=True, S=S, stopA=0, flags=()):
    NCH = S // 128
    NG = S // 512
    nc = bass.Bass("TRN2", target_bir_lowering=False)
    hT_d = nc.dram_tensor("hT", [D, S], BF16, kind="ExternalInput").ap()
    wA_d = nc.dram_tensor("wA", [D, 384], F32, kind="ExternalInput").ap()
    id_d = nc.dram_tensor("ident", [128, 128], BF16, kind="ExternalInput").ap()
    E_d = nc.dram_tensor("Emat", [64, S], BF16, kind="ExternalInput").ap()
    tri_d = nc.dram_tensor("tri", [128, 128], BF16, kind="ExternalInput").ap()
    caus_d = nc.dram_tensor("caus", [128, 128], F32, kind="ExternalInput").ap()
    btab_d = nc.dram_tensor("btab", [128, 128], F32, kind="ExternalInput").ap()
    negb_d = nc.dram_tensor("negb", [128, 4], F32, kind="ExternalInput").ap()
    yT_d = nc.dram_tensor("yT", [192, S], BF16, kind="ExternalOutput").ap()
    wT_d = nc.dram_tensor("wT", [D, 512], F32, kind="ExternalInput").ap()
    wR_d = nc.dram_tensor("wR", [D, 322], F32, kind="ExternalInput").ap()
    tabs_d = nc.dram_tensor("tabs", [4, 64, S], F32, kind="ExternalInput").ap()
    cw_d = nc.dram_tensor("cw", [64, 10], F32, kind="ExternalInput").ap()
    misc_d = nc.dram_tensor("misc", [128, 8], F32, kind="ExternalInput").ap()
    wcol_d = nc.dram_tensor("wcol", [128, 1], F32, kind="ExternalInput").ap()
    trif_d = nc.dram_tensor("trif", [128, 128], F32, kind="ExternalInput").ap()
    idf_d = nc.dram_tensor("identf", [128, 128], F32, kind="ExternalInput").ap()
    hT_v = hT_d.rearrange("(mc p) t -> p mc t", p=128)
    gC_d = nc.dram_tensor("gC_scratch", [S // 128, 128, 128], BF16, kind="Internal").ap()
    lo = slice(0, 64)
    hi = slice(64, 128)

    with contextlib.ExitStack() as st:
        P = Prog(nc, st)
        b_y = Buf()
        ident = P.sb("ident_sb", [128, 128], BF16); b_id = Buf()
        P.dma(ident[:], id_d, writes=[b_id])
        tri = P.sb("tri_sb", [128, 128], BF16); b_tri = Buf()
        P.dma(tri[:], tri_d, writes=[b_tri])
        caus = P.sb("caus_sb", [128, 128], F32); b_caus = Buf()
        P.dma(caus[:], caus_d, writes=[b_caus])
        btab = P.sb("btab_sb", [128, 128], F32); b_btab = Buf()
        P.dma(btab[:], btab_d, writes=[b_btab])
        negb = P.sb("negb_sb", [128, 4], F32); b_negb = Buf()
        P.dma(negb[:], negb_d, writes=[b_negb])
        zeros = P.sb("zeros_sb", [128, 260], BF16); b_zero = Buf()
        P.op("dve", lambda e: e.memset(zeros[:], 0.0), writes=[b_zero])
        bank = [P.ps("bank%d" % i, [128, 512], F32) for i in range(6)]
        b_bank = [Buf(psum=True) for _ in range(6)]
        bankT = P.ps("bankT", [128, 1024], BF16); b_bankT = Buf(psum=True)
        hTt = [P.sb("hTt%d" % i, [128, 8, 512], BF16) for i in range(2)]
        b_hTt = [Buf() for _ in range(2)]
        wst = P.sb("wst", [128, 512], F32); b_wst = Buf()
        Wb = P.sb("Wb", [128, 8, 512], BF16); b_Wb = Buf()

        def load_w(dst, b_dst, src_d, ncols):
            for mc in range(8):
                P.dma(wst[:, 0:ncols], src_d[mc * 128:(mc + 1) * 128, :], writes=[b_wst])
                P.op("pool", lambda e, mc=mc: e.tensor_copy(out=dst[:, mc, 0:ncols], in_=wst[:, 0:ncols]),
                     reads=[b_wst], writes=[b_dst])

        QM = P.sb("QM", [128, S], BF16); b_QM = Buf()
        KE = P.sb("KE", [128, S], BF16); b_KE = Buf()
        VA = P.sb("VA", [128, NCH, 65], BF16); b_VA = Buf()
        P.op("dve", lambda e: e.memset(VA[:, :, 64:65], 1.0), writes=[b_VA])
        sgA = P.sb("sgA", [128, NCH, 64], BF16); b_sgA = Buf()

        if do_a:
            wA = Wb
            load_w(wA, b_Wb, wA_d, 384)
            if "noE" not in flags:
                P.dma(KE[hi, :], E_d, writes=[b_KE])
            kmean = P.sb("kmean", [128, 64], F32); b_km = Buf()
            P.op("dve", lambda e: e.memset(kmean[:], 0.0), writes=[b_km])
            q32 = [P.sb("q32_%d" % i, [128, 512], F32) for i in range(2)]
            b_q32 = [Buf() for _ in range(2)]
            gm = [P.sb("gm%d" % i, [128, 64], F32) for i in range(2)]
            sel = [P.sb("sel%d" % i, [128, 64], F32) for i in range(2)]
            top8 = [P.sb("top8_%d" % i, [128, 8], F32) for i in range(2)]
            thr = [P.sb("thr%d" % i, [128, 1], F32) for i in range(2)]
            negm = [P.sb("negm%d" % i, [128, 128], BF16) for i in range(2)]
            b_gm = [Buf() for _ in range(2)]
            b_sel = [Buf() for _ in range(2)]
            b_top8 = [Buf() for _ in range(2)]
            b_thr = [Buf() for _ in range(2)]
            b_negm = [Buf() for _ in range(2)]
            for i in range(2):
                P.op("dve", lambda e, i=i: e.memset(negm[i][:], 0.0), writes=[b_negm[i]])

            for i in range(NG):
                hb = i % 2
                tsl = slice(i * 512, (i + 1) * 512)
                P.dma(hTt[hb][:], hT_v[:, :, tsl], writes=[b_hTt[hb]])
                for mc in range(8 if "nok" not in flags else 0):
                    P.op("pe", lambda e, mc=mc, hb=hb: e.matmul(bank[1][:, :], lhsT=wA[:, mc, 128:256],
                                                               rhs=hTt[hb][:, mc, :], start=(mc == 0), stop=(mc == 7)),
                         reads=[b_Wb, b_hTt[hb]], writes=[b_bank[1]])
                if "nok" not in flags and "nokc" not in flags:
                  P.op("act", lambda e, tsl=tsl: e.copy(out=KE[lo, tsl], in_=bank[1][lo, :]),
                     reads=[b_bank[1]], writes=[b_KE])
                if "nok" not in flags and "nored" not in flags:
                  P.op("dve", lambda e, i=i: e.tensor_reduce(
                    out=kmean[lo, 2 * i:2 * i + 2], in_=bank[1][lo, :].rearrange("p (b t) -> p b t", b=2),
                    axis=AX.X, op=ALU.add),
                    reads=[b_bank[1]], writes=[b_km])
                for mc in range(8 if "noq" not in flags else 0):
                    P.op("pe", lambda e, mc=mc, hb=hb: e.matmul(bank[0][:, :], lhsT=wA[:, mc, 0:128],
                                                               rhs=hTt[hb][:, mc, :], start=(mc == 0), stop=(mc == 7)),
                         reads=[b_Wb, b_hTt[hb]], writes=[b_bank[0]])
                if "noq" not in flags:
                  P.op("act", lambda e, tsl=tsl: e.mul(out=QM[lo, tsl], in_=bank[0][lo, :], mul=0.125),
                     reads=[b_bank[0]], writes=[b_QM])
                qb = i % 2
                if "noq" not in flags:
                  P.op("dve", lambda e, qb=qb: e.tensor_copy(out=q32[qb][:], in_=bank[0][:, :]),
                     reads=[b_bank[0]], writes=[b_q32[qb]])
                for sub in range(4 if "norow" not in flags else 0):
                    ch = 4 * i + sub
                    rb = 2 + sub % 2
                    for mc in range(8):
                        P.op("pe", lambda e, mc=mc, hb=hb, sub=sub, rb=rb: e.matmul(
                            bank[rb][:, 0:128], lhsT=hTt[hb][:, mc, sub * 128:(sub + 1) * 128],
                            rhs=wA[:, mc, 256:384], start=(mc == 0), stop=(mc == 7)),
                            reads=[b_Wb, b_hTt[hb]], writes=[b_bank[rb]])
                    P.op("dve", lambda e, ch=ch, rb=rb: e.tensor_copy(out=VA[:, ch, 0:64], in_=bank[rb][:, 0:64]),
                         reads=[b_bank[rb]], writes=[b_VA])
                    P.op("act", lambda e, ch=ch, rb=rb: e.activation(out=sgA[:, ch, :], in_=bank[rb][:, 64:128],
                                                                       func=AF.Silu),
                         reads=[b_bank[rb]], writes=[b_sgA])
                for sub in range(4 if stopA != 1 else 0):
                    ch = 4 * i + sub
                    j = ch // 2
                    sb_ = ch % 2
                    P.op("pe", lambda e, qb=qb, sub=sub: e.matmul(
                        bank[4][:, 0:64], lhsT=q32[qb][:, sub * 128:(sub + 1) * 128], rhs=kmean[:, :],
                        start=True, stop=True),
                        reads=[b_q32[qb], b_km], writes=[b_bank[4]])
                    P.op("dve", lambda e, sb_=sb_, j=j: e.tensor_tensor(
                        out=gm[sb_][:], in0=bank[4][:, 0:64], in1=caus[:, 64 - j:128 - j], op=ALU.add),
                        reads=[b_bank[4], b_caus], writes=[b_gm[sb_]])
                    P.op("dve", lambda e, sb_=sb_: e.max(out=top8[sb_][:], in_=gm[sb_][:]),
                         reads=[b_gm[sb_]], writes=[b_top8[sb_]])
                    P.op("dve", lambda e, sb_=sb_: e.tensor_scalar(
                        out=thr[sb_][:], in0=top8[sb_][:, 2:3], scalar1=-1e29, scalar2=None, op0=ALU.max),
                        reads=[b_top8[sb_]], writes=[b_thr[sb_]])
                    P.op("dve", lambda e, sb_=sb_: e.tensor_scalar(
                        out=sel[sb_][:], in0=gm[sb_][:], scalar1=thr[sb_][:, 0:1], scalar2=None, op0=ALU.is_ge),
                        reads=[b_gm[sb_], b_thr[sb_]], writes=[b_sel[sb_]])
                    P.op("dve", lambda e, sb_=sb_, j=j: e.memset(sel[sb_][:, j:j + 1], 1.0),
                         reads=[b_sel[sb_]], writes=[b_sel[sb_]])
                    P.op("dve", lambda e, sb_=sb_, sub=sub: e.tensor_scalar(
                        out=negm[sb_][:, 64:128], in0=sel[sb_][:], scalar1=BIG, scalar2=negb[:, sub:sub + 1],
                        op0=ALU.mult, op1=ALU.add),
                        reads=[b_sel[sb_], b_negb], writes=[b_negm[sb_]])
                    tcol = (ch % 8) * 128
                    P.op("pe", lambda e, sb_=sb_, tcol=tcol: e.transpose(
                        out=bankT[:, tcol:tcol + 128], in_=negm[sb_][:], identity=ident[:]),
                        reads=[b_negm[sb_], b_id], writes=[b_bankT])
                    P.op("act", lambda e, ch=ch, tcol=tcol: e.copy(
                        out=QM[hi, ch * 128:(ch + 1) * 128], in_=bankT[hi, tcol:tcol + 128]),
                        reads=[b_bankT], writes=[b_QM])

            NPT = 3
            PT = [P.sb("PT%d" % i, [128, 512], BF16) for i in range(NPT)]
            b_PT = [Buf() for _ in range(NPT)]
            ya = [P.sb("ya%d" % i, [128, 4, 64], BF16) for i in range(2)]
            b_ya = [Buf() for _ in range(2)]
            rec = [P.sb("rec%d" % i, [128, 4], F32) for i in range(2)]
            b_rec = [Buf() for _ in range(2)]
            yTs = [P.sb("yTsA%d" % i, [128, 256], BF16) for i in range(2)]
            b_yTs = [Buf() for _ in range(2)]
            tiles = [(g, c) for g in range(NG if stopA == 0 else 0) for c in range(4 * g + 4)]
            b_pvtok = Buf()

            def emit_qk(k):
                g, c = tiles[k]
                r = c - 4 * g
                qlo = 0 if r <= 0 else 128 * r
                sb_ = k % 2
                pb = k % NPT
                delta = 4 * g - c
                P.op("pe", lambda e: e.matmul(
                    bank[sb_][:, qlo:512], lhsT=KE[:, c * 128:(c + 1) * 128],
                    rhs=QM[:, g * 512 + qlo:(g + 1) * 512], start=True, stop=True),
                    reads=[b_KE, b_QM], writes=[b_bank[sb_]])
                P.op("act", lambda e: e.activation(
                    out=PT[pb][:, qlo:512], in_=bank[sb_][:, qlo:512], func=AF.Exp,
                    bias=btab[:, delta + 3:delta + 4], scale=1.0),
                    reads=[b_bank[sb_], b_btab, b_pvtok], writes=[b_PT[pb]])
                if r >= 0:
                    P.op("pool", lambda e: e.tensor_tensor(
                        out=PT[pb][:, qlo:qlo + 128], in0=PT[pb][:, qlo:qlo + 128], in1=tri[:], op=ALU.mult),
                        reads=[b_PT[pb], b_tri], writes=[b_PT[pb]])

            def emit_pv(k):
                g, c = tiles[k]
                r = c - 4 * g
                qlo = 0 if r <= 0 else 128 * r
                pb = k % NPT
                nb = 2 + g % 2
                if c == 0:
                    P.op("pe", lambda e: e.matmul(bank[nb][:, 0:260], lhsT=zeros[:, 0:128], rhs=zeros[:, 0:260],
                                                  start=True, stop=False, skip_group_check=True),
                         reads=[b_zero], writes=[b_bank[nb]])
                for sub in range(qlo // 128, 4):
                    last = (c == 4 * g + sub)
                    P.op("pe", lambda e, sub=sub, last=last: e.matmul(
                        bank[nb][:, sub * 65:(sub + 1) * 65], lhsT=PT[pb][:, sub * 128:(sub + 1) * 128],
                        rhs=VA[:, c, :], start=False, stop=last, skip_group_check=True),
                        reads=[b_PT[pb], b_VA], writes=[b_bank[nb], b_pvtok])
                if c != 4 * g + 3:
                    return
                yb_ = g % 2
                ndv = bank[nb][:, 0:260].rearrange("p (s e) -> p s e", e=65)
                P.op("dve", lambda e: e.reciprocal(out=rec[yb_][:], in_=ndv[:, :, 64]),
                     reads=[b_bank[nb]], writes=[b_rec[yb_]])
                for sub in range(4):
                    P.op("dve", lambda e, sub=sub: e.scalar_tensor_tensor(
                        out=ya[yb_][:, sub, :], in0=bank[nb][:, sub * 65:sub * 65 + 64], scalar=rec[yb_][:, sub:sub + 1],
                        in1=sgA[:, 4 * g + sub, :], op0=ALU.mult, op1=ALU.mult),
                        reads=[b_bank[nb], b_rec[yb_], b_sgA], writes=[b_ya[yb_]])
                for pr in range(2):
                    P.op("pe", lambda e, pr=pr: e.transpose(
                        out=bankT[:, pr * 128:(pr + 1) * 128],
                        in_=ya[yb_][:, 2 * pr:2 * pr + 2, :].rearrange("p s e -> p (s e)"), identity=ident[:]),
                        reads=[b_ya[yb_], b_id], writes=[b_bankT])
                P.op("act", lambda e: e.copy(out=yTs[yb_][:], in_=bankT[:, 0:256]),
                     reads=[b_bankT], writes=[b_yTs[yb_]])
                dst = yT_d[0:64, g * 512:(g + 1) * 512].rearrange("e (p s q) -> s e p q", p=2, s=2)
                for s2 in range(2):
                    P.dma(dst[s2], yTs[yb_][s2 * 64:(s2 + 1) * 64, :].rearrange("e (p q) -> e p q", p=2),
                          reads=[b_yTs[yb_]], writes=[b_y])

            if tiles:
                emit_qk(0)
            for k in range(len(tiles)):
                if k + 1 < len(tiles):
                    emit_qk(k + 1)
                emit_pv(k)

        if do_bc:
            NT2 = S // 256
            QT, b_QT, KT, b_KT = QM, b_QM, KE, b_KE
            VC, b_VC, sgB, b_sgB = VA, b_VA, sgA, b_sgA
            wT = Wb
            load_w(wT, b_Wb, wT_d, 512)
            wR = P.sb("wR_sb", [128, 8, 322], BF16); b_wR = Buf()
            load_w(wR, b_wR, wR_d, 322)
            cw = P.sb("cw_sb", [64, 10], F32); b_cw = Buf()
            P.dma(cw[:], cw_d, writes=[b_cw])
            misc = P.sb("misc_sb", [128, 8], F32); b_misc = Buf()
            P.dma(misc[:], misc_d, writes=[b_misc])
            wcol = P.sb("wcol_sb", [128, 1], F32); b_wcol = Buf()
            P.dma(wcol[:], wcol_d, writes=[b_wcol])
            trif = P.sb("trif_sb", [128, 128], F32); b_trif = Buf()
            P.dma(trif[:], trif_d, writes=[b_trif])
            identf = P.sb("identf_sb", [128, 128], F32); b_idf = Buf()
            P.dma(identf[:], idf_d, writes=[b_idf])
            onesf = P.sb("onesf_sb", [128, 128], F32); b_ones = Buf()
            P.op("dve", lambda e: e.memset(onesf[:], 1.0), writes=[b_ones])
            VB = P.sb("VB", [128, NCH, 64], BF16); b_VB = Buf()
            gst = [P.sb("gst%d" % i, [128, 2, 128], BF16) for i in range(2)]
            b_gst = [Buf() for _ in range(2)]
            b_gCd = Buf()
            G = P.sb("G", [128, 2, NCH], F32); b_G = Buf()
            TB = [P.sb("TB%d" % i, [128, 4, 256], F32) for i in range(1)] * 2
            b_TB = [Buf()] * 2
            tmpB = [P.sb("tmpB%d" % i, [128, 256], F32) for i in range(2)]
            b_tmpB = [Buf() for _ in range(2)]
            cst = [[P.sb("cst%d_%d" % (a, i), [64, 260], F32) for i in range(2)] for a in range(2)]
            b_cst = [[Buf() for _ in range(2)] for _ in range(2)]
            acc = [P.sb("acc%d" % a, [64, 256], F32) for a in range(2)]
            b_acc = [Buf() for _ in range(2)]
            sgk = P.sb("sgk", [64, 256], F32); b_sgk = Buf()
            for a in range(2):
                P.op("dve", lambda e, a=a: e.memset(cst[a][0][:, 0:3], 0.0), writes=[b_cst[a][0]])
            tabs_v = tabs_d.rearrange("k d t -> d k t")

            for i in range(NT2):
                hb = i % 2
                tb = i % 2
                tsl = slice(i * 256, (i + 1) * 256)
                P.dma(hTt[hb][:, :, 0:256], hT_v[:, :, tsl], writes=[b_hTt[hb]])
                P.dma(TB[tb][hi, :, :], tabs_v[:, :, tsl], writes=[b_TB[tb]])
                for grp in range(4):
                    for mc in range(8):
                        P.op("pe", lambda e, mc=mc, hb=hb, grp=grp: e.matmul(
                            bank[grp][:, 0:256], lhsT=wT[:, mc, grp * 128:(grp + 1) * 128], rhs=hTt[hb][:, mc, 0:256],
                            start=(mc == 0), stop=(mc == 7)),
                            reads=[b_Wb, b_hTt[hb]], writes=[b_bank[grp]])
                for (dst, b_dst, g0, g1, t0) in ((QT, b_QT, 0, 2, 0), (KT, b_KT, 1, 3, 2)):
                    P.op("dve", lambda e, g0=g0, tb=tb, t0=t0: e.tensor_tensor(
                        out=tmpB[0][hi, :], in0=bank[g0][hi, 0:256], in1=TB[tb][hi, t0, :], op=ALU.mult),
                        reads=[b_bank[g0], b_TB[tb]], writes=[b_tmpB[0]])
                    P.op("dve", lambda e, g1=g1, tb=tb, t0=t0: e.tensor_tensor(
                        out=tmpB[1][hi, :], in0=bank[g1][hi, 0:256], in1=TB[tb][hi, t0 + 1, :], op=ALU.mult),
                        reads=[b_bank[g1], b_TB[tb]], writes=[b_tmpB[1]])
                    P.op("pool", lambda e, dst=dst, tsl=tsl: e.tensor_tensor(
                        out=dst[hi, tsl], in0=tmpB[0][hi, :], in1=tmpB[1][hi, :], op=ALU.add),
                        reads=[b_tmpB[0], b_tmpB[1]], writes=[b_dst])
                cb = i % 2
                for a in range(2):
                    P.op("act", lambda e, a=a, cb=cb: e.copy(out=cst[a][cb][:, 3:259], in_=bank[a][lo, 0:256]),
                         reads=[b_bank[a]], writes=[b_cst[a][cb]])
                    if i > 0:
                        P.op("pool", lambda e, a=a, cb=cb: e.tensor_copy(out=cst[a][cb][:, 0:3],
                                                                        in_=cst[a][1 - cb][:, 256:259]),
                             reads=[b_cst[a][1 - cb]], writes=[b_cst[a][cb]])
                    P.op("pool", lambda e, a=a, cb=cb: e.tensor_scalar(
                        out=acc[a][:], in0=cst[a][cb][:, 0:256], scalar1=cw[:, 4 * a:4 * a + 1],
                        scalar2=cw[:, 8 + a:9 + a], op0=ALU.mult, op1=ALU.add),
                        reads=[b_cst[a][cb], b_cw], writes=[b_acc[a]])
                    for jj in range(1, 4):
                        P.op("dve", lambda e, a=a, cb=cb, jj=jj: e.scalar_tensor_tensor(
                            out=acc[a][:], in0=cst[a][cb][:, jj:jj + 256], scalar=cw[:, 4 * a + jj:4 * a + jj + 1],
                            in1=acc[a][:], op0=ALU.mult, op1=ALU.add),
                            reads=[b_cst[a][cb], b_cw, b_acc[a]], writes=[b_acc[a]])
                P.op("act", lambda e, tsl=tsl: e.activation(out=QT[lo, tsl], in_=acc[0][:], func=AF.Silu),
                     reads=[b_acc[0]], writes=[b_QT])
                P.op("act", lambda e: e.activation(out=sgk[:], in_=acc[1][:], func=AF.Sigmoid),
                     reads=[b_acc[1]], writes=[b_sgk])
                P.op("dve", lambda e, tsl=tsl: e.scalar_tensor_tensor(
                    out=KT[lo, tsl], in0=acc[1][:], scalar=0.125, in1=sgk[:], op0=ALU.mult, op1=ALU.mult),
                    reads=[b_acc[1], b_sgk], writes=[b_KT])
                for sub in range(2):
                    ch = 2 * i + sub
                    rb = 4 + sub % 2
                    for mc in range(8):
                        P.op("pe", lambda e, mc=mc, hb=hb, sub=sub, rb=rb: e.matmul(
                            bank[rb][:, 0:322], lhsT=hTt[hb][:, mc, sub * 128:(sub + 1) * 128],
                            rhs=wR[:, mc, :], start=(mc == 0), stop=(mc == 7)),
                            reads=[b_wR, b_hTt[hb]], writes=[b_bank[rb]])
                    P.op("dve", lambda e, ch=ch, rb=rb: e.tensor_copy(out=VB[:, ch, :], in_=bank[rb][:, 0:64]),
                         reads=[b_bank[rb]], writes=[b_VB])
                    P.op("act", lambda e, ch=ch, rb=rb: e.activation(out=sgB[:, ch, :], in_=bank[rb][:, 64:128],
                                                                       func=AF.Silu),
                         reads=[b_bank[rb]], writes=[b_sgB])
                    P.op("dve", lambda e, ch=ch, rb=rb: e.tensor_copy(out=VC[:, ch, 0:64], in_=bank[rb][:, 128:192]),
                         reads=[b_bank[rb]], writes=[b_VC])
                    P.op("act", lambda e, sub=sub, tb=tb, rb=rb: e.activation(out=gst[tb][:, sub, 0:64],
                                                                              in_=bank[rb][:, 192:256], func=AF.Sigmoid),
                         reads=[b_bank[rb]], writes=[b_gst[tb]])
                    P.op("act", lambda e, sub=sub, tb=tb, rb=rb: e.activation(out=gst[tb][:, sub, 64:128],
                                                                              in_=bank[rb][:, 256:320], func=AF.Silu),
                         reads=[b_bank[rb]], writes=[b_gst[tb]])
                    P.op("dve", lambda e, ch=ch, rb=rb: e.tensor_copy(out=G[:, :, ch], in_=bank[rb][:, 320:322]),
                         reads=[b_bank[rb]], writes=[b_G])
                P.dma(gC_d[2 * i:2 * i + 2].rearrange("c p e -> p c e"), gst[tb][:], reads=[b_gst[tb]], writes=[b_gCd])

            cz = P.sb("cz", [128, 10, NCH], F32); b_cz = [Buf() for _ in range(10)]
            rz = P.sb("rz", [128, 6, NCH + 1], F32); b_rz = [Buf() for _ in range(6)]
            Acol = P.sb("Acol", [128, 1], F32); b_Acol = Buf()
            Abc = P.sb("Abc", [128, 128], F32); b_Abc = Buf()
            P.op("dve", lambda e: e.memset(Abc[:], 0.0), writes=[b_Abc])
            P.op("dve", lambda e: e.tensor_scalar(out=cz[:, 0, :], in0=G[:, 1, :], scalar1=misc[:, 2:3], scalar2=None,
                                                  op0=ALU.add), reads=[b_G, b_misc], writes=[b_cz[0]])
            P.op("act", lambda e: e.activation(out=cz[:, 1, :], in_=cz[:, 0, :], func=AF.Exp, scale=-1.0),
                 reads=[b_cz[0]], writes=[b_cz[1]])
            P.op("act", lambda e: e.activation(out=cz[:, 2, :], in_=cz[:, 1, :], func=AF.Ln, bias=1.0),
                 reads=[b_cz[1]], writes=[b_cz[2]])
            P.op("pe", lambda e: e.matmul(bank[0][:, 0:NCH], lhsT=trif[:], rhs=cz[:, 2, :], start=True, stop=True),
                 reads=[b_trif, b_cz[2]], writes=[b_bank[0]])
            P.op("dve", lambda e: e.tensor_copy(out=cz[:, 3, :], in_=bank[0][:, 0:NCH]),
                 reads=[b_bank[0]], writes=[b_cz[3]])
            P.op("dve", lambda e: e.scalar_tensor_tensor(out=cz[:, 4, :], in0=G[:, 0, :], scalar=misc[:, 1:2],
                                                         in1=cz[:, 3, :], op0=ALU.add, op1=ALU.add),
                 reads=[b_G, b_misc, b_cz[3]], writes=[b_cz[4]])
            P.op("pe", lambda e: e.transpose(out=bank[1][0:NCH, 0:128], in_=cz[:, 4, :], identity=identf[:]),
                 reads=[b_cz[4], b_idf], writes=[b_bank[1]])
            P.op("dve", lambda e: e.tensor_reduce(out=Acol[0:NCH, :], in_=bank[1][0:NCH, 0:128], axis=AX.X, op=ALU.max),
                 reads=[b_bank[1]], writes=[b_Acol])
            P.op("dve", lambda e: e.tensor_scalar(out=Abc[0:NCH, :], in0=Abc[0:NCH, :], scalar1=0.0,
                                                  scalar2=Acol[0:NCH, 0:1], op0=ALU.mult, op1=ALU.add),
                 reads=[b_Acol, b_Abc], writes=[b_Abc])
            P.op("pe", lambda e: e.transpose(out=bank[2][:, 0:128], in_=Abc[:, :], identity=identf[:]),
                 reads=[b_Abc, b_idf], writes=[b_bank[2]])
            P.op("dve", lambda e: e.tensor_copy(out=rz[:, 0, 0:NCH], in_=bank[2][:, 0:NCH]),
                 reads=[b_bank[2]], writes=[b_rz[0]])
            P.op("pe", lambda e: e.matmul(bank[3][:, 0:NCH], lhsT=onesf[:], rhs=cz[:, 2, :], start=True, stop=True),
                 reads=[b_ones, b_cz[2]], writes=[b_bank[3]])
            P.op("dve", lambda e: e.tensor_scalar(out=rz[:, 1, 0:NCH], in0=bank[3][:, 0:NCH], scalar1=-1.0,
                                                  scalar2=None, op0=ALU.mult),
                 reads=[b_bank[3]], writes=[b_rz[1]])
            P.op("dve", lambda e: e.memset(rz[:, 2, 0:1], 0.0), writes=[b_rz[2]])
            P.op("dve", lambda e: e.tensor_tensor_scan(out=rz[:, 2, 1:NCH + 1], data0=rz[:, 0, 0:NCH],
                                                       data1=rz[:, 1, 0:NCH], initial=0.0, op0=ALU.max, op1=ALU.add),
                 reads=[b_rz[0], b_rz[1], b_rz[2]], writes=[b_rz[2]])
            P.op("dve", lambda e: e.tensor_tensor(out=rz[:, 3, 0:NCH], in0=rz[:, 2, 0:NCH], in1=rz[:, 0, 0:NCH],
                                                  op=ALU.max),
                 reads=[b_rz[2], b_rz[0]], writes=[b_rz[3]])
            P.op("dve", lambda e: e.tensor_tensor(out=rz[:, 4, 0:NCH], in0=rz[:, 2, 0:NCH], in1=rz[:, 3, 0:NCH],
                                                  op=ALU.subtract),
                 reads=[b_rz[2], b_rz[3]], writes=[b_rz[4]])
            P.op("act", lambda e: e.activation(out=rz[:, 5, 0:NCH], in_=rz[:, 4, 0:NCH], func=AF.Exp),
                 reads=[b_rz[4]], writes=[b_rz[5]])
            P.op("dve", lambda e: e.tensor_tensor(out=cz[:, 5, :], in0=cz[:, 4, :], in1=rz[:, 3, 0:NCH], op=ALU.subtract),
                 reads=[b_cz[4], b_rz[3]], writes=[b_cz[5]])
            P.op("act", lambda e: e.activation(out=cz[:, 6, :], in_=cz[:, 5, :], func=AF.Exp),
                 reads=[b_cz[5]], writes=[b_cz[6]])
            P.op("dve", lambda e: e.tensor_tensor(out=cz[:, 7, :], in0=cz[:, 3, :], in1=rz[:, 3, 0:NCH], op=ALU.subtract),
                 reads=[b_cz[3], b_rz[3]], writes=[b_cz[7]])
            P.op("act", lambda e: e.activation(out=cz[:, 8, :], in_=cz[:, 7, :], func=AF.Exp),
                 reads=[b_cz[7]], writes=[b_cz[8]])
            for ch in range(NCH):
                P.op("pool", lambda e, ch=ch: e.tensor_scalar(out=VC[:, ch, :], in0=VC[:, ch, :],
                                                               scalar1=cz[:, 6, ch:ch + 1], scalar2=None, op0=ALU.mult),
                     reads=[b_VC, b_cz[6]], writes=[b_VC])

            Krow = [P.sb("Krow%d" % i, [128, 128], BF16) for i in range(2)]
            b_Krow = [Buf() for _ in range(2)]
            qbt = [P.sb("qbt%d" % i, [128, 128], BF16) for i in range(2)]
            qct = [P.sb("qct%d" % i, [128, 128], BF16) for i in range(2)]
            b_qbt = [Buf() for _ in range(2)]
            b_qct = [Buf() for _ in range(2)]
            Cbf = [P.sb("Cbf%d" % i, [128, 65], BF16) for i in range(2)]
            Rbf = [P.sb("Rbf%d" % i, [128, 64], BF16) for i in range(2)]
            b_Cbf = [Buf() for _ in range(2)]
            b_Rbf = [Buf() for _ in range(2)]
            Cst = [P.sb("Cst%d" % i, [64, 65], F32) for i in range(2)]
            b_Cst = [Buf() for _ in range(2)]
            Rst = [P.sb("Rst%d" % i, [128, 64], F32) for i in range(2)]
            b_Rst = [Buf() for _ in range(2)]
            kvc = P.sb("kvc", [64, 65], F32); b_kvc = Buf()
            kvb = P.sb("kvb", [128, 64], F32); b_kvb = Buf()
            for i in range(2):
                P.op("dve", lambda e, i=i: e.memset(qbt[i][:], 0.0), writes=[b_qbt[i]])
                P.op("dve", lambda e, i=i: e.memset(qct[i][:], 0.0), writes=[b_qct[i]])
                P.op("dve", lambda e, i=i: e.memset(Cbf[i][:], 0.0), writes=[b_Cbf[i]])
                P.op("dve", lambda e, i=i: e.memset(Rbf[i][:], 0.0), writes=[b_Rbf[i]])
            P.op("dve", lambda e: e.memset(Cst[0][:], 0.0), writes=[b_Cst[0]])
            P.op("dve", lambda e: e.memset(Rst[0][:], 0.0), writes=[b_Rst[0]])
            STm = [P.sb("STm%d" % i, [128, 128], BF16) for i in range(4)]
            b_STm = [Buf() for _ in range(4)]
            st6 = [P.sb("st6_%d" % i, [128, 6], F32) for i in range(2)]
            mv = [P.sb("mv%d" % i, [128, 4], F32) for i in range(2)]
            t1 = [P.sb("t1_%d" % i, [128, 64], F32) for i in range(2)]
            hs = P.sb("hs", [128, 64], F32); b_hs = Buf()
            dm = P.sb("dm", [128, 2], F32); b_dm = Buf()
            yn = [P.sb("yn%d" % i, [128, 128], BF16) for i in range(2)]
            b_st6 = [Buf() for _ in range(2)]
            b_mv = [Buf() for _ in range(2)]
            b_t1 = [Buf() for _ in range(2)]
            b_yn = [Buf() for _ in range(2)]
            yTsBC = [P.sb("yTsBC%d" % i, [128, 512], BF16) for i in range(1)] * 2
            b_yTsBC = [Buf()] * 2
            gct = [P.sb("gct%d" % i, [128, 128], BF16) for i in range(2)]
            b_gct = [Buf() for _ in range(2)]
            stm_n = [0]

            def head_norm(src_ap, b_src, gate_ap, b_gate, k, ynk, col0):
                P.op("dve", lambda e: e.bn_stats(out=st6[k][:], in_=src_ap), reads=[b_src], writes=[b_st6[k]])
                P.op("dve", lambda e: e.bn_aggr(out=mv[k][:, 0:2], in_=st6[k][:]), reads=[b_st6[k]], writes=[b_mv[k]])
                P.op("dve", lambda e: e.tensor_scalar(out=mv[k][:, 2:3], in0=mv[k][:, 1:2], scalar1=1e-5, scalar2=None,
                                                      op0=ALU.add), reads=[b_mv[k]], writes=[b_mv[k]])
                P.op("act", lambda e: e.activation(out=mv[k][:, 3:4], in_=mv[k][:, 2:3], func=AF.Sqrt),
                     reads=[b_mv[k]], writes=[b_mv[k]])
                P.op("dve", lambda e: e.reciprocal(out=mv[k][:, 2:3], in_=mv[k][:, 3:4]),
                     reads=[b_mv[k]], writes=[b_mv[k]])
                P.op("dve", lambda e: e.scalar_tensor_tensor(out=t1[k][:], in0=src_ap, scalar=mv[k][:, 0:1],
                                                             in1=gate_ap, op0=ALU.subtract, op1=ALU.mult),
                     reads=[b_src, b_mv[k], b_gate], writes=[b_t1[k]])
                P.op("dve", lambda e: e.tensor_scalar(out=yn[ynk][:, col0:col0 + 64], in0=t1[k][:], scalar1=mv[k][:, 2:3],
                                                      scalar2=None, op0=ALU.mult),
                     reads=[b_t1[k], b_mv[k]], writes=[b_yn[ynk]])

            for ch in range(NCH):
                kb = ch % 2
                csl = slice(ch * 128, (ch + 1) * 128)
                cbk, nt = ch // 2, ch % 2
                P.dma(gct[kb][:], gC_d[ch], reads=[b_gCd], writes=[b_gct[kb]])
                P.op("pool", lambda e, kb=kb, csl=csl: e.tensor_copy(out=qbt[kb][hi, :], in_=QT[hi, csl]),
                     reads=[b_QT], writes=[b_qbt[kb]])
                P.op("pool", lambda e, kb=kb, csl=csl: e.tensor_copy(out=qct[kb][lo, :], in_=QT[lo, csl]),
                     reads=[b_QT], writes=[b_qct[kb]])
                si = stm_n[0] % 4
                stm_n[0] += 1
                pb_ = si % 2
                P.op("pe", lambda e, pb_=pb_, csl=csl, kb=kb: e.matmul(bank[pb_][:, 0:128], lhsT=KT[:, csl], rhs=qct[kb][:],
                                                                      start=True, stop=True),
                     reads=[b_KT, b_qct[kb]], writes=[b_bank[pb_]])
                P.op("dve", lambda e, si=si, pb_=pb_: e.tensor_tensor(out=STm[si][:], in0=bank[pb_][:, 0:128], in1=tri[:],
                                                                    op=ALU.mult),
                     reads=[b_bank[pb_], b_tri], writes=[b_STm[si]])
                P.op("pe", lambda e, kb=kb: e.matmul(bank[2][:, 0:65], lhsT=qct[kb][:], rhs=Cbf[kb][:],
                                                    start=True, stop=False),
                     reads=[b_qct[kb], b_Cbf[kb]], writes=[b_bank[2]])
                P.op("pe", lambda e, si=si, ch=ch: e.matmul(bank[2][:, 0:65], lhsT=STm[si][:], rhs=VC[:, ch, :],
                                                           start=False, stop=True),
                     reads=[b_STm[si], b_VC], writes=[b_bank[2]])
                P.op("dve", lambda e: e.tensor_scalar(out=dm[:, 1:2], in0=bank[2][:, 64:65], scalar1=-1.0, scalar2=None,
                                                      op0=ALU.mult), reads=[b_bank[2]], writes=[b_dm])
                P.op("dve", lambda e, ch=ch: e.scalar_tensor_tensor(
                    out=dm[:, 0:1], in0=bank[2][:, 64:65], scalar=cz[:, 8, ch:ch + 1], in1=dm[:, 1:2],
                    op0=ALU.max, op1=ALU.max), reads=[b_bank[2], b_cz[8], b_dm], writes=[b_dm])
                P.op("dve", lambda e: e.reciprocal(out=dm[:, 1:2], in_=dm[:, 0:1]), reads=[b_dm], writes=[b_dm])
                P.op("dve", lambda e, ch=ch, kb=kb: e.scalar_tensor_tensor(
                    out=hs[:], in0=bank[2][:, 0:64], scalar=dm[:, 1:2], in1=gct[kb][:, 0:64], op0=ALU.mult, op1=ALU.mult),
                    reads=[b_bank[2], b_dm, b_gct[kb]], writes=[b_hs])
                head_norm(hs[:], b_hs, gct[kb][:, 64:128], b_gct[kb], 1, kb, 64)
                used = []
                for mt in range(nt + 1):
                    tk = 2 * cbk + mt
                    ksl = slice(tk * 128, (tk + 1) * 128)
                    si = stm_n[0] % 4
                    stm_n[0] += 1
                    pb_ = si % 2
                    P.op("pe", lambda e, pb_=pb_, ksl=ksl, kb=kb: e.matmul(
                        bank[pb_][:, 0:128], lhsT=KT[:, ksl], rhs=qbt[kb][:], start=True, stop=True),
                        reads=[b_KT, b_qbt[kb]], writes=[b_bank[pb_]])
                    if mt == nt:
                        P.op("dve", lambda e, si=si, pb_=pb_: e.tensor_tensor(out=STm[si][:], in0=bank[pb_][:, 0:128],
                                                                            in1=tri[:], op=ALU.mult),
                             reads=[b_bank[pb_], b_tri], writes=[b_STm[si]])
                    else:
                        P.op("act", lambda e, si=si, pb_=pb_: e.copy(out=STm[si][:], in_=bank[pb_][:, 0:128]),
                             reads=[b_bank[pb_]], writes=[b_STm[si]])
                    used.append((si, tk))
                rk = cbk % 2
                P.op("pe", lambda e, kb=kb, rk=rk: e.matmul(bank[3][:, 0:64], lhsT=qbt[kb][:], rhs=Rbf[rk][:],
                                                           start=True, stop=False),
                     reads=[b_qbt[kb], b_Rbf[rk]], writes=[b_bank[3]])
                for n_, (si, tk) in enumerate(used):
                    P.op("pe", lambda e, si=si, tk=tk, last=(n_ == len(used) - 1): e.matmul(
                        bank[3][:, 0:64], lhsT=STm[si][:], rhs=VB[:, tk, :], start=False, stop=last),
                        reads=[b_STm[si], b_VB], writes=[b_bank[3]])
                head_norm(bank[3][:, 0:64], b_bank[3], sgB[:, ch, :], b_sgB, 0, kb, 0)
                sub = ch % 4
                yb_ = (ch // 4) % 2
                P.op("pe", lambda e, kb=kb, sub=sub: e.transpose(out=bankT[:, sub * 128:(sub + 1) * 128], in_=yn[kb][:],
                                                                identity=ident[:]),
                     reads=[b_yn[kb], b_id], writes=[b_bankT])
                P.op("dve", lambda e, yb_=yb_, sub=sub: e.tensor_scalar(
                    out=yTsBC[yb_][:, sub * 128:(sub + 1) * 128], in0=bankT[:, sub * 128:(sub + 1) * 128],
                    scalar1=wcol[:, 0:1], scalar2=None, op0=ALU.mult),
                    reads=[b_bankT, b_wcol], writes=[b_yTsBC[yb_]])
                if sub == 3:
                    g = ch // 4
                    P.dma(yT_d[64:192, g * 512:(g + 1) * 512], yTsBC[yb_][:], reads=[b_yTsBC[yb_]], writes=[b_y])
                tcol = 512 + (ch % 4) * 128
                P.op("pe", lambda e, tcol=tcol, csl=csl: e.transpose(out=bankT[:, tcol:tcol + 128], in_=KT[:, csl],
                                                                   identity=ident[:]),
                     reads=[b_KT, b_id], writes=[b_bankT])
                P.op("act", lambda e, kb=kb, tcol=tcol: e.copy(out=Krow[kb][:], in_=bankT[:, tcol:tcol + 128]),
                     reads=[b_bankT], writes=[b_Krow[kb]])
                if ch + 1 < NCH:
                    P.op("pe", lambda e, kb=kb, ch=ch: e.matmul(bank[4][:, 0:65], lhsT=Krow[kb][:], rhs=VC[:, ch, :],
                                                               start=True, stop=True),
                         reads=[b_Krow[kb], b_VC], writes=[b_bank[4]])
                    o_, n2 = ch % 2, (ch + 1) % 2
                    P.op("dve", lambda e, ch=ch: e.tensor_scalar(out=kvc[:], in0=bank[4][lo, 0:65],
                                                                 scalar1=rz[lo, 5, ch + 1:ch + 2], scalar2=None,
                                                                 op0=ALU.mult),
                         reads=[b_bank[4], b_rz[5]], writes=[b_kvc])
                    P.op("dve", lambda e, ch=ch, o_=o_, n2=n2: e.scalar_tensor_tensor(
                        out=Cst[n2][:], in0=Cst[o_][:], scalar=rz[lo, 5, ch + 1:ch + 2], in1=kvc[:],
                        op0=ALU.mult, op1=ALU.add),
                        reads=[b_Cst[o_], b_rz[5], b_kvc], writes=[b_Cst[n2]])
                    P.op("pool", lambda e, n2=n2: e.tensor_copy(out=Cbf[n2][lo, :], in_=Cst[n2][:]),
                         reads=[b_Cst[n2]], writes=[b_Cbf[n2]])
                if cbk + 1 < NCH // 2:
                    P.op("pe", lambda e, kb=kb, ch=ch, nt=nt: e.matmul(bank[5][:, 0:64], lhsT=Krow[kb][:], rhs=VB[:, ch, :],
                                                                      start=(nt == 0), stop=(nt == 1)),
                         reads=[b_Krow[kb], b_VB], writes=[b_bank[5]])
                    if nt == 1:
                        o_, n2 = cbk % 2, (cbk + 1) % 2
                        P.op("dve", lambda e: e.tensor_scalar(out=kvb[hi, :], in0=bank[5][hi, 0:64], scalar1=misc[hi, 0:1],
                                                              scalar2=None, op0=ALU.mult),
                             reads=[b_bank[5], b_misc], writes=[b_kvb])
                        P.op("dve", lambda e, o_=o_, n2=n2: e.scalar_tensor_tensor(
                            out=Rst[n2][hi, :], in0=Rst[o_][hi, :], scalar=misc[hi, 0:1], in1=kvb[hi, :],
                            op0=ALU.mult, op1=ALU.add),
                            reads=[b_Rst[o_], b_misc, b_kvb], writes=[b_Rst[n2]])
                        P.op("pool", lambda e, n2=n2: e.tensor_copy(out=Rbf[n2][hi, :], in_=Rst[n2][hi, :]),
                             reads=[b_Rst[n2]], writes=[b_Rbf[n2]])

        P.finish([b_y])
    return nc


def alibi_slope(h):
    return float(2.0 ** (-8.0 * (h + 1) / 8))


def pb_consts(c, S=S):
    slope = alibi_slope(c)
    p = np.arange(128, dtype=np.float64)
    E = np.zeros((64, S), np.float32)
    E[np.arange(S) // 256, np.arange(S)] = 1.0
    tri = (p[:, None] <= p[None, :]).astype(np.float32)
    caus = np.zeros((128, 128), np.float32)
    caus[:, 64:] = -1e30
    deltas = np.arange(-3, 125, dtype=np.float64)
    btab = slope * (p[:, None] - 128.0 * deltas[None, :])
    negb = -BIG - slope * (128.0 * np.arange(4)[None, :] + p[:, None])
    return {
        "ident": _ident(), "Emat": E.astype(NPBF), "tri": tri.astype(NPBF), "caus": caus,
        "btab": btab.astype(np.float32), "negb": negb.astype(np.float32),
    }


def pb_consts_bc(c, S=S):
    hb = c % 4
    gamma = 1.0 - 2.0 ** (-5.0 - hb)
    L = 256
    theta = (1.0 / (10000.0 ** np.linspace(0.0, 1.0, 32, dtype=np.float32))).astype(np.float32)
    pos = np.arange(S, dtype=np.float32)
    ang = pos[None, :] * np.concatenate([theta, theta])[:, None]
    cos, sin = np.cos(ang).astype(np.float64), np.sin(ang).astype(np.float64)
    sgn = np.concatenate([-np.ones(32), np.ones(32)])[:, None]
    n = (np.arange(S) % L).astype(np.float64)[None, :]
    gq = gamma ** n
    gk = gamma ** (-n) / 8.0
    tabs = np.stack([cos * gq, sgn * sin * gq, cos * gk, sgn * sin * gk]).astype(np.float32)
    p = np.arange(128)
    trif = (p[:, None] <= p[None, :]).astype(np.float32)
    return {"tabs": tabs, "trif": trif, "identf": np.eye(128, dtype=np.float32), "gammaL": np.float32(gamma ** L)}


def pb_inputs(c, hT, w_in_l, conv_w_l, conv_b_l, gate_bias_l, ret_w_l, ml_w_l, S=S, do_a=True, do_bc=True):
    m = {"hT": hT}
    m.update(pb_consts(c, S=S))
    r = np.arange(64)
    if do_a:
        cols = np.concatenate([c * 64 + r + off for off in (0, 512, 512, 0, 1024, 1536)])
        m["wA"] = np.ascontiguousarray(w_in_l[:, cols])
    else:
        m["wA"] = np.zeros((D, 384), np.float32)
    if do_bc:
        h = c % 4
        B0, C0 = 2048, 3072
        bq, bk, bv, bg = [B0 + k * 256 + h * 64 + r for k in range(4)]
        cq, ck, cv, co, cg = [C0 + k * 256 + h * 64 + r for k in range(5)]
        ci = np.array([C0 + 5 * 256 + h]); cf = np.array([C0 + 5 * 256 + 4 + h])
        sw = np.concatenate([r[32:], r[:32]])
        colsT = np.concatenate([cq, bq, ck, bk, cq, bq[sw], ck, bk[sw]])
        colsR = np.concatenate([bv, bg, cv, co, cg, ci, cf])
        m["wT"] = np.ascontiguousarray(w_in_l[:, colsT])
        m["wR"] = np.ascontiguousarray(w_in_l[:, colsR])
        cb = pb_consts_bc(c, S=S)
        gl = cb.pop("gammaL")
        m.update(cb)
        cw = np.zeros((64, 10), np.float32)
        cw[:, 0:4] = conv_w_l[:, h * 64 + r].T
        cw[:, 4:8] = conv_w_l[:, 256 + h * 64 + r].T
        cw[:, 8] = conv_b_l[h * 64 + r]
        cw[:, 9] = conv_b_l[256 + h * 64 + r]
        m["cw"] = cw
        misc = np.zeros((128, 8), np.float32)
        misc[:, 0] = gl
        misc[:, 1] = gate_bias_l[h]
        misc[:, 2] = gate_bias_l[4 + h]
        m["misc"] = misc
        m["wcol"] = np.concatenate([ret_w_l[h * 64 + r], ml_w_l[h * 64 + r]])[:, None].astype(np.float32)
    return m


_PROGS = {}


def _prog(name, fn):
    if name not in _PROGS:
        _PROGS[name] = fn()
    return _PROGS[name]


def _run(nc, maps):
    return run_bass_kernel_spmd(nc, maps, core_ids=list(range(NCORES))).results


def kernel(x, norm_w, w_in, conv_w, conv_b, gate_bias, ret_norm_w, mlstm_norm_w, w_out, final_norm_w):
    f32 = np.float32
    x2 = np.ascontiguousarray(np.asarray(x, f32)[0])
    norm_w, w_in, conv_w, conv_b = (np.asarray(a, f32) for a in (norm_w, w_in, conv_w, conv_b))
    gate_bias, ret_norm_w, mlstm_norm_w, w_out = (np.asarray(a, f32) for a in (gate_bias, ret_norm_w, mlstm_norm_w, w_out))
    final_norm_w = np.asarray(final_norm_w, f32)
    ident = _ident()
    xs = [x2[c * TOK:(c + 1) * TOK] for c in range(NCORES)]
    res = _run(_prog("pa_first", lambda: build_pa(True, False)),
               [{"x": xs[c], "nw": norm_w[0][None, :], "ident": ident} for c in range(NCORES)])
    hT = np.concatenate([r["hT"] for r in res], axis=1)
    out = None
    for l in range(DEPTH):
        res = _run(_prog("pb", lambda: build_pb(True, True)),
                   [pb_inputs(c, hT, w_in[l], conv_w[l], conv_b[l], gate_bias[l], ret_norm_w[l], mlstm_norm_w[l])
                    for c in range(NCORES)])
        yT = np.concatenate([res[c]["yT"][0:64] for c in range(8)]
                            + [res[h]["yT"][64:128] for h in range(4)]
                            + [res[4 + h]["yT"][128:192] for h in range(4)], axis=0)
        last = l == DEPTH - 1
        nw = final_norm_w if last else norm_w[l + 1]
        maps = [{"x": xs[c], "nw": nw[None, :], "ident": ident,
                 "yT": np.ascontiguousarray(yT[:, c * TOK:(c + 1) * TOK]), "wo": w_out[l]} for c in range(NCORES)]
        if last:
            res = _run(_prog("pa_final", lambda: build_pa(False, True)), maps)
            out = np.concatenate([r["out"] for r in res], axis=0)
        else:
            res = _run(_prog("pa_mid", lambda: build_pa(False, False)), maps)
            xs = [r["xo"] for r in res]
            hT = np.concatenate([r["hT"] for r in res], axis=1)
    return out[None].astype(np.float32)
```
